# Optimizing a Trainium2 kernel written in Bass

```python
import math
import jax, jax.numpy as jnp
from jax import lax
import numpy as np

D_MODEL = 1024
BATCH = 4
SEQ = 4096
DEPTH = 2

CHUNK = 64
D_MIX = D_MODEL
D_ATTN = D_MIX // 2
D_RG = D_MIX // 4
D_S5 = D_MIX - D_ATTN - D_RG
ATTN_HEAD_DIM = 64
ATTN_HEADS = D_ATTN // ATTN_HEAD_DIM
N_PREV_CHUNKS = 8
BAND = (N_PREV_CHUNKS + 1) * CHUNK
REL_CLIP = 128
RG_CONV_WIDTH = 4
RG_BLOCKS = 4
RG_BLOCK_DIM = D_RG // RG_BLOCKS
RG_C = 8.0
S5_GROUP_DIM = 16
S5_GROUPS = D_S5 // S5_GROUP_DIM
S5_STATE = 64
D_IN = 3 * D_ATTN + 2 * D_RG + D_S5
FFN_DIM = 2816
N_EXPERTS = 8
TOP_K = 2
EXPERT_DIM = 2816
EXPERT_BLOCK = 128
N_DENSE = (DEPTH + 1) // 2
N_MOE = DEPTH // 2
EPS = 1e-6

kernel_name = "hybrid_chunk_attn_rglru_s5_moe"


def _rmsnorm(x, g):
    xf = x.astype(jnp.float32)
    var = jnp.mean(xf * xf, axis=-1, keepdims=True)
    return (xf * lax.rsqrt(var + EPS) * g.astype(jnp.float32)).astype(x.dtype)


def _linear_scan(a, b):
    def op(c1, c2):
        a1, b1 = c1
        a2, b2 = c2
        return a1 * a2, a2 * b1 + b2
    return lax.associative_scan(op, (a, b), axis=1)[1]


def _chunked_attention(q, k, v, rel_bias):
    b, l, h, dh = q.shape
    nc = l // CHUNK
    qc = q.reshape(b, nc, CHUNK, h, dh)
    pad = ((0, 0), (N_PREV_CHUNKS, 0), (0, 0), (0, 0), (0, 0))
    kp = jnp.pad(k.reshape(b, nc, CHUNK, h, dh), pad)
    vp = jnp.pad(v.reshape(b, nc, CHUNK, h, dh), pad)
    band_idx = jnp.arange(nc)[:, None] + jnp.arange(N_PREV_CHUNKS + 1)[None, :]
    kb = kp[:, band_idx].reshape(b, nc, BAND, h, dh)
    vb = vp[:, band_idx].reshape(b, nc, BAND, h, dh)
    s = jnp.einsum('bcqhd,bckhd->bhcqk', qc, kb).astype(jnp.float32) * (dh ** -0.5)
    qpos = jnp.arange(CHUNK)[:, None] + N_PREV_CHUNKS * CHUNK
    kpos = jnp.arange(BAND)[None, :]
    rel = jnp.clip(qpos - kpos, -REL_CLIP, REL_CLIP) + REL_CLIP
    bias = rel_bias.astype(jnp.float32)[:, rel]
    key_pos = jnp.arange(nc)[:, None] * CHUNK + jnp.arange(BAND)[None, :] - N_PREV_CHUNKS * CHUNK
    valid = key_pos >= 0
    s = s + bias[:, None]
    s = jnp.where(valid[None, None, :, None, :], s, -1e30)
    p = jax.nn.softmax(s, axis=-1).astype(v.dtype)
    o = jnp.einsum('bhcqk,bckhd->bcqhd', p, vb)
    return o.reshape(b, l, h * dh)


def _rglru_branch(xr, gate, conv_w, conv_b, wx, bx, wa, ba, lam):
    b, l, c = xr.shape
    xc = lax.conv_general_dilated(
        xr, conv_w[:, None, :].astype(xr.dtype), window_strides=(1,),
        padding=[(RG_CONV_WIDTH - 1, 0)], dimension_numbers=('NWC', 'WIO', 'NWC'),
        feature_group_count=c) + conv_b.astype(xr.dtype)
    xf = xc.astype(jnp.float32)
    xblk = xf.reshape(b, l, RG_BLOCKS, RG_BLOCK_DIM)
    gx = jax.nn.sigmoid(jnp.einsum('blhi,hij->blhj', xblk, wx.astype(jnp.float32)).reshape(b, l, c)
                        + bx.astype(jnp.float32))
    ga = jax.nn.sigmoid(jnp.einsum('blhi,hij->blhj', xblk, wa.astype(jnp.float32)).reshape(b, l, c)
                        + ba.astype(jnp.float32))
    log_a = -RG_C * ga * jax.nn.softplus(-lam.astype(jnp.float32))
    a = jnp.exp(log_a)
    mult = jnp.sqrt(-jnp.expm1(2.0 * log_a))
    hseq = _linear_scan(a, mult * gx * xf)
    y = hseq * jax.nn.gelu(gate.astype(jnp.float32))
    return y.astype(xr.dtype)


def _s5_branch(u, a_re, a_im, log_dt, b_re, b_im, c_re, c_im, d, w_glu):
    bsz, l, _ = u.shape
    uf = u.astype(jnp.float32).reshape(bsz, l, S5_GROUPS, S5_GROUP_DIM)
    A = lax.complex(a_re.astype(jnp.float32), a_im.astype(jnp.float32))
    dt = jnp.exp(log_dt.astype(jnp.float32))[:, None]
    A_bar = jnp.exp(A * dt)
    Bm = lax.complex(b_re.astype(jnp.float32), b_im.astype(jnp.float32))
    Cm = lax.complex(c_re.astype(jnp.float32), c_im.astype(jnp.float32))
    B_bar = ((A_bar - 1.0) / A)[..., None] * Bm
    Bu = jnp.einsum('blgc,gpc->blgp', uf.astype(jnp.complex64), B_bar)
    A_seq = jnp.broadcast_to(A_bar, Bu.shape)
    states = _linear_scan(A_seq, Bu)
    y = jnp.real(jnp.einsum('blgp,gcp->blgc', states, Cm)) \
        + d.astype(jnp.float32).reshape(S5_GROUPS, S5_GROUP_DIM) * uf
    y = jax.nn.gelu(y.reshape(bsz, l, D_S5))
    y = y * jax.nn.sigmoid(y @ w_glu.astype(jnp.float32))
    return y.astype(u.dtype)


def _swiglu(h, w1, w3, w2):
    return (jax.nn.silu(h @ w1) * (h @ w3)) @ w2


def _moe(x2, router, w1, w3, w2):
    n, dm = x2.shape
    logits = x2.astype(jnp.float32) @ router.astype(jnp.float32)
    top_logit, top_e = lax.top_k(logits, TOP_K)
    gates = jax.nn.softmax(top_logit, axis=-1)
    flat_e = top_e.reshape(-1)
    flat_tok = jnp.repeat(jnp.arange(n, dtype=jnp.int32), TOP_K)
    flat_g = gates.reshape(-1)
    order = jnp.argsort(flat_e)
    se, st, sg = flat_e[order], flat_tok[order], flat_g[order]
    counts = jnp.zeros((N_EXPERTS,), jnp.int32).at[flat_e].add(1)
    starts = jnp.cumsum(counts) - counts
    pcounts = (counts + EXPERT_BLOCK - 1) // EXPERT_BLOCK * EXPERT_BLOCK
    pends = jnp.cumsum(pcounts)
    pstarts = pends - pcounts
    n_assign = n * TOP_K
    rank = jnp.arange(n_assign, dtype=jnp.int32) - starts[se]
    dest = pstarts[se] + rank
    n_pad = (n_assign + EXPERT_BLOCK - 1) // EXPERT_BLOCK * EXPERT_BLOCK + N_EXPERTS * EXPERT_BLOCK
    n_blocks = n_pad // EXPERT_BLOCK
    buf_tok = jnp.full((n_pad,), n, jnp.int32).at[dest].set(st)
    buf_g = jnp.zeros((n_pad,), jnp.float32).at[dest].set(sg)
    blk_start = jnp.arange(n_blocks, dtype=jnp.int32) * EXPERT_BLOCK
    blk_e = jnp.clip(jnp.searchsorted(pends, blk_start, side='right'), 0, N_EXPERTS - 1)
    x_pad = jnp.concatenate([x2, jnp.zeros((1, dm), x2.dtype)], axis=0)
    xb = x_pad[buf_tok].reshape(n_blocks, EXPERT_BLOCK, dm)

    def expert_block(args):
        xblk, e = args
        return _swiglu(xblk, w1[e], w3[e], w2[e])

    yb = lax.map(expert_block, (xb, blk_e)).reshape(n_pad, dm)
    yb = yb * buf_g[:, None].astype(yb.dtype)
    out = jnp.zeros((n + 1, dm), x2.dtype).at[buf_tok].add(yb)
    return out[:n]


def setup_inputs(seed: int = 0) -> dict:
    key = jax.random.key(seed)
    ks = iter(jax.random.split(key, 40))

    def nrm(shape, scale):
        return jax.random.normal(next(ks), shape, jnp.float32) * scale

    def gain(shape):
        return 1.0 + nrm(shape, 0.02)

    x = nrm((BATCH, SEQ, D_MODEL), 1.0)
    norm_mix_g = gain((DEPTH, D_MODEL))
    w_in = nrm((DEPTH, D_MODEL, D_IN), D_MODEL ** -0.5)
    attn_rel_bias = nrm((DEPTH, ATTN_HEADS, 2 * REL_CLIP + 1), 0.1)
    rg_conv_w = nrm((DEPTH, RG_CONV_WIDTH, D_RG), RG_CONV_WIDTH ** -0.5)
    rg_conv_b = nrm((DEPTH, D_RG), 0.01)
    rg_wx = nrm((DEPTH, RG_BLOCKS, RG_BLOCK_DIM, RG_BLOCK_DIM), RG_BLOCK_DIM ** -0.5)
    rg_bx = nrm((DEPTH, D_RG), 0.01)
    rg_wa = nrm((DEPTH, RG_BLOCKS, RG_BLOCK_DIM, RG_BLOCK_DIM), RG_BLOCK_DIM ** -0.5)
    rg_ba = nrm((DEPTH, D_RG), 0.01)
    a_pow = jax.random.uniform(next(ks), (DEPTH, D_RG), jnp.float32, 0.9, 0.999)
    a0 = a_pow ** (1.0 / RG_C)
    rg_lambda = jnp.log(a0) - jnp.log1p(-a0)
    s5_a_re = -0.5 + nrm((DEPTH, S5_GROUPS, S5_STATE), 0.01)
    s5_a_im = math.pi * jnp.arange(S5_STATE, dtype=jnp.float32) + nrm((DEPTH, S5_GROUPS, S5_STATE), 0.01)
    s5_log_dt = jax.random.uniform(next(ks), (DEPTH, S5_GROUPS), jnp.float32,
                                   math.log(1e-3), math.log(1e-1))
    s5_b_re = nrm((DEPTH, S5_GROUPS, S5_STATE, S5_GROUP_DIM), (2 * S5_GROUP_DIM) ** -0.5)
    s5_b_im = nrm((DEPTH, S5_GROUPS, S5_STATE, S5_GROUP_DIM), (2 * S5_GROUP_DIM) ** -0.5)
    s5_c_re = nrm((DEPTH, S5_GROUPS, S5_GROUP_DIM, S5_STATE), S5_STATE ** -0.5)
    s5_c_im = nrm((DEPTH, S5_GROUPS, S5_GROUP_DIM, S5_STATE), S5_STATE ** -0.5)
    s5_d = nrm((DEPTH, D_S5), 1.0)
    s5_w_glu = nrm((DEPTH, D_S5, D_S5), D_S5 ** -0.5)
    g_group = gain((DEPTH, D_MIX))
    w_out = nrm((DEPTH, D_MIX, D_MODEL), D_MIX ** -0.5)
    norm_ffn_g = gain((DEPTH, D_MODEL))
    ffn_w1 = nrm((N_DENSE, D_MODEL, FFN_DIM), D_MODEL ** -0.5)
    ffn_w3 = nrm((N_DENSE, D_MODEL, FFN_DIM), D_MODEL ** -0.5)
    ffn_w2 = nrm((N_DENSE, FFN_DIM, D_MODEL), FFN_DIM ** -0.5)
    moe_router = nrm((N_MOE, D_MODEL, N_EXPERTS), D_MODEL ** -0.5)
    moe_w1 = nrm((N_MOE, N_EXPERTS, D_MODEL, EXPERT_DIM), D_MODEL ** -0.5)
    moe_w3 = nrm((N_MOE, N_EXPERTS, D_MODEL, EXPERT_DIM), D_MODEL ** -0.5)
    moe_w2 = nrm((N_MOE, N_EXPERTS, EXPERT_DIM, D_MODEL), EXPERT_DIM ** -0.5)
    final_norm_g = gain((D_MODEL,))
    return {
        "x": x, "norm_mix_g": norm_mix_g, "w_in": w_in, "attn_rel_bias": attn_rel_bias,
        "rg_conv_w": rg_conv_w, "rg_conv_b": rg_conv_b, "rg_wx": rg_wx, "rg_bx": rg_bx,
        "rg_wa": rg_wa, "rg_ba": rg_ba, "rg_lambda": rg_lambda,
        "s5_a_re": s5_a_re, "s5_a_im": s5_a_im, "s5_log_dt": s5_log_dt,
        "s5_b_re": s5_b_re, "s5_b_im": s5_b_im, "s5_c_re": s5_c_re, "s5_c_im": s5_c_im,
        "s5_d": s5_d, "s5_w_glu": s5_w_glu, "g_group": g_group, "w_out": w_out,
        "norm_ffn_g": norm_ffn_g, "ffn_w1": ffn_w1, "ffn_w3": ffn_w3, "ffn_w2": ffn_w2,
        "moe_router": moe_router, "moe_w1": moe_w1, "moe_w3": moe_w3, "moe_w2": moe_w2,
        "final_norm_g": final_norm_g,
    }


def reference(x, norm_mix_g, w_in, attn_rel_bias, rg_conv_w, rg_conv_b, rg_wx, rg_bx, rg_wa, rg_ba,
              rg_lambda, s5_a_re, s5_a_im, s5_log_dt, s5_b_re, s5_b_im, s5_c_re, s5_c_im, s5_d,
              s5_w_glu, g_group, w_out, norm_ffn_g, ffn_w1, ffn_w3, ffn_w2, moe_router, moe_w1,
              moe_w3, moe_w2, final_norm_g):
    bsz, l, dm = x.shape
    split_at = [D_ATTN, 2 * D_ATTN, 3 * D_ATTN, 3 * D_ATTN + D_RG, 3 * D_ATTN + 2 * D_RG]
    for layer in range(DEPTH):
        u = _rmsnorm(x, norm_mix_g[layer])
        proj = u @ w_in[layer]
        q, k, v, xr, gate, us = jnp.split(proj, split_at, axis=-1)
        hs = (bsz, l, ATTN_HEADS, ATTN_HEAD_DIM)
        y_attn = _chunked_attention(q.reshape(hs), k.reshape(hs), v.reshape(hs), attn_rel_bias[layer])
        y_rg = _rglru_branch(xr, gate, rg_conv_w[layer], rg_conv_b[layer], rg_wx[layer], rg_bx[layer],
                             rg_wa[layer], rg_ba[layer], rg_lambda[layer])
        y_s5 = _s5_branch(us, s5_a_re[layer], s5_a_im[layer], s5_log_dt[layer], s5_b_re[layer],
                          s5_b_im[layer], s5_c_re[layer], s5_c_im[layer], s5_d[layer], s5_w_glu[layer])
        gg = g_group[layer]
        mixed = jnp.concatenate([
            _rmsnorm(y_attn, gg[:D_ATTN]),
            _rmsnorm(y_rg, gg[D_ATTN:D_ATTN + D_RG]),
            _rmsnorm(y_s5, gg[D_ATTN + D_RG:]),
        ], axis=-1)
        x = x + mixed @ w_out[layer]
        h = _rmsnorm(x, norm_ffn_g[layer])
        idx = layer // 2
        if layer % 2 == 0:
            y = _swiglu(h, ffn_w1[idx], ffn_w3[idx], ffn_w2[idx])
        else:
            y = _moe(h.reshape(-1, dm), moe_router[idx], moe_w1[idx], moe_w3[idx],
                     moe_w2[idx]).reshape(bsz, l, dm)
        x = x + y
    return _rmsnorm(x, final_norm_g)
```

```python
import contextlib
import numpy as np
import concourse.bass as bass
import concourse.mybir as mybir
from concourse.bass_utils import run_bass_kernel_spmd

F32 = mybir.dt.float32
BF16 = mybir.dt.bfloat16
U32 = mybir.dt.uint32
AF = mybir.ActivationFunctionType
ALU = mybir.AluOpType
AX = mybir.AxisListType

L = 4096
D = 1024
TT = 512
NT = L // TT
NS = L // 128
DIN = 2304
FF = 2816
NF = FF // 128
NE = 8
EPS = 1e-6
NEG = -30000.0

COMPUTE = ("pe", "act", "dve", "pool")
QUEUES = ("sp", "act", "pool")
DMA_RING = {"sp": 16, "act": 8, "pool": 4}


class Reg:
    __slots__ = ("name", "lw", "rs", "children", "parent")

    def __init__(self, name, parent=None):
        self.name = name
        self.lw = None
        self.rs = []
        self.children = []
        self.parent = parent
        if parent is not None:
            parent.children.append(self)

    def related(self):
        yield self
        if self.parent is not None:
            yield self.parent
        for c in self.children:
            yield c

    def sub(self, name):
        return Reg(self.name + "." + str(name), self)


class Op:
    __slots__ = ("id", "eng", "fn", "deps", "is_dma", "signal", "semval", "semidx", "qidx")

    def __init__(self, id, eng, fn, is_dma):
        self.id = id
        self.eng = eng
        self.fn = fn
        self.is_dma = is_dma
        self.deps = set()
        self.signal = False
        self.semval = None
        self.semidx = None
        self.qidx = None


class Prog:
    def __init__(self, nc):
        self.nc = nc
        self.ops = []
        self.by_eng = {e: [] for e in ("pe", "act", "dve", "pool", "sp")}
        self.ndma = {e: 0 for e in QUEUES}
        self.dma_ops = {e: [] for e in QUEUES}
        self.pending = {}

    def _add(self, eng, fn, reads, writes, is_dma):
        op = Op(len(self.ops), eng, fn, is_dma)
        deps = set()
        for r in reads:
            for x in r.related():
                if x.lw is not None:
                    deps.add(x.lw)
        for w in writes:
            for x in w.related():
                if x.lw is not None:
                    deps.add(x.lw)
                deps.update(x.rs)
        if eng in self.pending:
            deps |= self.pending.pop(eng)
        latest = {}
        for d in deps:
            dop = self.ops[d]
            if dop.is_dma:
                op.deps.add(d)
                continue
            if (not is_dma) and dop.eng == eng and eng == "pe":
                continue
            if latest.get(dop.eng, -1) < d:
                latest[dop.eng] = d
        op.deps.update(latest.values())
        for r in reads:
            r.rs.append(op.id)
        for w in writes:
            w.lw = op.id
            w.rs = []
            for c in w.children:
                c.lw = op.id
                c.rs = []
        if is_dma:
            op.qidx = self.ndma[eng]
            self.ndma[eng] += 1
            self.dma_ops[eng].append(op)
        self.ops.append(op)
        self.by_eng[eng].append(op)
        return op

    def op(self, eng, fn, reads=(), writes=()):
        return self._add(eng, fn, list(reads), list(writes), False)

    def dma(self, eng, out, in_, reads=(), writes=(), **kw):
        return self._add(eng, lambda e: e.dma_start(out=out, in_=in_, **kw), list(reads), list(writes), True)

    def barrier(self):
        b = set()
        for e in COMPUTE:
            lst = [o for o in self.by_eng[e] if not o.is_dma]
            if lst:
                b.add(lst[-1].id)
        for q in QUEUES:
            for o in self.dma_ops[q][-DMA_RING[q]:]:
                b.add(o.id)
        for e in self.by_eng:
            self.pending[e] = set(b) | self.pending.get(e, set())

    def emit(self, final_wait_ops=()):
        nc = self.nc
        ops = self.ops
        for op in ops:
            for d in op.deps:
                ops[d].signal = True
        for fo in final_wait_ops:
            fo.signal = True
        es = contextlib.ExitStack()
        sems = {e: es.enter_context(nc.semaphore("s_" + e)) for e in COMPUTE}
        dsems = {}
        for q in QUEUES:
            if self.ndma[q] > 0:
                dsems[q] = [es.enter_context(nc.semaphore("d_%s%d" % (q, i))) for i in range(DMA_RING[q])]
        cnt = {e: 0 for e in COMPUTE}
        for op in ops:
            if op.is_dma:
                op.semidx = op.qidx % DMA_RING[op.eng]
                op.semval = 16 * (op.qidx // DMA_RING[op.eng] + 1)
            elif op.signal:
                cnt[op.eng] += 1
                op.semval = cnt[op.eng]
        engobj = {"pe": "tensor", "act": "scalar", "dve": "vector", "pool": "gpsimd", "sp": "sync"}
        block = es.enter_context(nc.Block())

        def make_body(ename):
            def body(e):
                waited = {}
                for op in self.by_eng[ename]:
                    need = {}
                    for d in op.deps:
                        dop = ops[d]
                        key = ("d", dop.eng, dop.semidx) if dop.is_dma else ("c", dop.eng)
                        if need.get(key, 0) < dop.semval:
                            need[key] = dop.semval
                    if op.is_dma and op.qidx >= DMA_RING[op.eng]:
                        key = ("d", op.eng, op.semidx)
                        v = op.semval - 16
                        if need.get(key, 0) < v:
                            need[key] = v
                    for key, v in need.items():
                        if waited.get(key, 0) >= v:
                            continue
                        waited[key] = v
                        if key[0] == "d":
                            e.wait_ge(dsems[key[1]][key[2]], v)
                        else:
                            e.wait_ge(sems[key[1]], v)
                    ins = op.fn(e)
                    if op.is_dma:
                        ins.then_inc(dsems[op.eng][op.semidx], 16)
                    elif op.signal:
                        ins.then_inc(sems[op.eng], 1)
                if ename == "sp":
                    for fo in final_wait_ops:
                        if fo.is_dma:
                            e.wait_ge(dsems[fo.eng][fo.semidx], fo.semval)
                        else:
                            e.wait_ge(sems[fo.eng], fo.semval)
            return body

        for ename in ("sp", "act", "dve", "pool", "pe"):
            if not self.by_eng[ename] and ename != "sp":
                continue
            getattr(block, engobj[ename])(make_body(ename))
        es.close()


class T:
    __slots__ = ("ap", "reg")

    def __init__(self, ap, reg):
        self.ap = ap
        self.reg = reg


class Arena:
    def __init__(self, t, cap):
        self.t = t
        self.cap = cap
        self.top = 0
        self.n = 0

    def carve(self, free, dt, name=None):
        n = 1
        for f in free:
            n *= f
        sz = 2 if dt == BF16 else 4
        nb = (n * sz + 63) // 64 * 64
        off = self.top
        self.top += nb
        assert self.top <= self.cap, ("SBUF arena overflow", self.top, self.cap, name)
        v = self.t[:, off // 4:(off + nb) // 4]
        if dt != F32:
            v = v.bitcast(dt)
        v = v[:, 0:n]
        if len(free) > 1:
            names = "abcde"[:len(free)]
            pat = "p (" + " ".join(names) + ") -> p " + " ".join(names)
            v = v.rearrange(pat, **{k: f for k, f in zip(names, free)})
        self.n += 1
        return T(v, Reg(name or ("t%d" % self.n)))


def _rows(ap2d, width):
    sh = ap2d.shape
    if len(sh) == 2:
        return ap2d.rearrange("a (b c) -> (a b) c", c=width)
    if len(sh) == 3:
        return ap2d.rearrange("l a (b c) -> (l a b) c", c=width)
    raise ValueError


def build(upto="all", dbg=()):
    nc = bass.Bass("TRN2", target_bir_lowering=False)
    es = contextlib.ExitStack()
    P = Prog(nc)

    def din(name, shape):
        return nc.dram_tensor(name, list(shape), F32, kind="ExternalInput").ap()

    def dscr(name, shape, dt, shared=False):
        kind = "ExternalOutput" if name in dbg else "Internal"
        if shared and kind == "Internal":
            return T(nc.dram_tensor(name, list(shape), dt, kind=kind, addr_space="Shared").ap(), Reg(name))
        return T(nc.dram_tensor(name, list(shape), dt, kind=kind).ap(), Reg(name))

    x_in = din("x", [L, D])
    norm_mix_g = din("norm_mix_g", [2, D])
    w_in = din("w_in", [2, D, DIN])
    biasT = din("biasT", [2, 8, 128, 640])
    rg_conv_w = din("rg_conv_w", [2, 4, 256])
    rg_conv_b = din("rg_conv_b", [2, 256])
    rg_wx = din("rg_wx", [2, 4, 64, 64])
    rg_bx = din("rg_bx", [2, 256])
    rg_wa = din("rg_wa", [2, 4, 64, 64])
    rg_ba = din("rg_ba", [2, 256])
    rg_lambda = din("rg_lambda", [2, 256])
    s5_a_re = din("s5_a_re", [2, 16, 64])
    s5_a_im = din("s5_a_im", [2, 16, 64])
    s5_log_dt = din("s5_log_dt", [2, 16])
    s5_b_re = din("s5_b_re", [2, 16, 64, 16])
    s5_b_im = din("s5_b_im", [2, 16, 64, 16])
    s5_c_re = din("s5_c_re", [2, 16, 16, 64])
    s5_c_im = din("s5_c_im", [2, 16, 16, 64])
    s5_d = din("s5_d", [2, 256])
    s5_w_glu = din("s5_w_glu", [2, 256, 256])
    g_group = din("g_group", [2, D])
    w_out = din("w_out", [2, D, D])
    norm_ffn_g = din("norm_ffn_g", [2, D])
    ffn_w1 = din("ffn_w1", [1, D, FF])
    ffn_w3 = din("ffn_w3", [1, D, FF])
    ffn_w2 = din("ffn_w2", [1, FF, D])
    moe_router = din("moe_router", [1, D, NE])
    moe_w1 = din("moe_w1", [1, NE, D, FF])
    moe_w3 = din("moe_w3", [1, NE, D, FF])
    moe_w2 = din("moe_w2", [1, NE, FF, D])
    final_norm_g = din("final_norm_g", [1, D])
    tok_idx = nc.dram_tensor("tok_idx", [128, 16], U32, kind="ExternalInput").ap()
    out_t = T(nc.dram_tensor("out", [L // 2, D], F32, kind="ExternalOutput").ap(), Reg("out"))

    xres = dscr("xres", [L, D], F32)
    rgsT = dscr("rgsT", [768, L], F32)
    mixedT = dscr("mixedT", [D, L], BF16)
    w_in_b = [dscr("w_in_b%d" % l, [D, DIN], BF16) for l in range(2)]
    w_out_b = [dscr("w_out_b%d" % l, [D, D], BF16) for l in range(2)]
    fw1_b = [dscr("fw1_b", [D, FF], BF16)]
    fw3_b = [dscr("fw3_b", [D, FF], BF16)]
    fw2_b = [dscr("fw2_b", [FF, D], BF16)]
    mw1_b = [dscr("mw1_b%d" % e, [D, FF], BF16) for e in range(NE)]
    mw3_b = [dscr("mw3_b%d" % e, [D, FF], BF16, shared=True) for e in range(NE)]
    mw2_b = [dscr("mw2_b%d" % e, [FF, D], BF16, shared=True) for e in range(NE)]

    ARENA_BYTES = 204 * 1024
    arena_t = es.enter_context(nc.sbuf_tensor("arena", [128, ARENA_BYTES // 4], F32))
    A = Arena(arena_t, ARENA_BYTES)
    banks = []
    for i in range(8):
        pt = es.enter_context(nc.psum_tensor("bank%d" % i, [128, 512], F32))
        banks.append(T(pt[:, :], Reg("bank%d" % i)))

    def bank_bf(i):
        return banks[i].ap.bitcast(BF16)

    ident_f = A.carve([128], F32, "ident_f")
    ones_f = A.carve([128], F32, "ones_f")
    ident_b = A.carve([128], BF16, "ident_b")
    P.op("pool", lambda e: e.memset(ident_f.ap, 1.0), writes=[ident_f.reg])
    P.op("pool", lambda e: e.affine_select(out=ident_f.ap, in_=ident_f.ap, pattern=[[-1, 128]],
                                           compare_op=ALU.is_equal, fill=0.0, base=0, channel_multiplier=1),
         reads=[ident_f.reg], writes=[ident_f.reg])
    P.op("pool", lambda e: e.memset(ones_f.ap, 1.0), writes=[ones_f.reg])
    P.op("dve", lambda e: e.tensor_copy(out=ident_b.ap, in_=ident_f.ap), reads=[ident_f.reg], writes=[ident_b.reg])
    def vec2(src_row, name):
        t = A.carve([2], F32, name)
        P.dma("sp", t.ap, src_row.rearrange("(ct p) -> p ct", p=128), writes=[t.reg], allow_slow_non_contiguous=True)
        return t

    rgp = []
    for l_ in range(2):
        cw_ = A.carve([2, 4], F32, "cw%d" % l_)
        for ct in range(2):
            P.dma("sp", cw_.ap[:, ct, :], rg_conv_w[l_][:, ct * 128:(ct + 1) * 128].rearrange("j p -> p j"), writes=[cw_.reg],
                  allow_slow_non_contiguous=True)
        rgp.append(dict(cw=cw_, cb=vec2(rg_conv_b[l_], "cb%d" % l_), bx=vec2(rg_bx[l_], "bx%d" % l_),
                        ba=vec2(rg_ba[l_], "ba%d" % l_), lam=vec2(rg_lambda[l_], "lam%d" % l_),
                        gg=vec2(g_group[l_, 512:768], "ggrg%d" % l_)))
    A_BASE = A.top

    def cast(dst, src, width, after=()):
        P.dma("pool", _rows(dst.ap, width), _rows(src, width), reads=list(after), writes=[dst.reg])

    cast(w_in_b[0], w_in[0], 1152)

    cast_queue = [(w_out_b[0], w_out[0], 1024), (fw1_b[0], ffn_w1[0], 1408), (fw3_b[0], ffn_w3[0], 1408),
                  (fw2_b[0], ffn_w2[0], 1024), (w_in_b[1], w_in[1], 1152), (w_out_b[1], w_out[1], 1024)]
    for e_ in range(NE):
        cast_queue += [(mw1_b[e_], moe_w1[0, e_], 1408), (mw3_b[e_], moe_w3[0, e_], 1408), (mw2_b[e_], moe_w2[0, e_], 1024)]

    def pop_cast(after=()):
        if cast_queue:
            d_, s_, w_ = cast_queue.pop(0)
            cast(d_, s_, w_, after=after)

    def issue_rest_casts(after=()):
        for _ in range(4):
            pop_cast(after=[rgsT.reg] + list(after))

    def rmsnorm_rows(xt, s, ss, sd, rstd, junk, nfeat):
        P.op("act", lambda e: e.activation(out=junk.ap, in_=xt.ap[:, s, :], func=AF.Square,
                                           accum_out=ss.ap[:, s:s + 1]),
             reads=[xt.reg], writes=[junk.reg, ss.reg])

    state = {"out_ops": []}

    def phase_A(l, xsrc, xsrc_reg):
        A.top = A_BASE
        qT = A.carve([4, L], BF16, "qT")
        kT = A.carve([4, L], BF16, "kT")
        vaug = A.carve([NS, 8, 65], BF16, "vaug")
        P.op("dve", lambda e: e.memset(vaug.ap, 1.0), writes=[vaug.reg])
        keep_top = A.top
        wsb = A.carve([8, DIN], BF16, "wsb")
        P.dma("sp", wsb.ap, w_in_b[l].ap.rearrange("(c p) n -> p c n", p=128), reads=[w_in_b[l].reg], writes=[wsb.reg])
        g_rep = A.carve([1, D], F32, "g_rep")
        P.dma("sp", g_rep.ap, norm_mix_g[l:l + 1, :].partition_broadcast(128), writes=[g_rep.reg])
        xts = [A.carve([4, D], F32, "xt%d" % i) for i in range(1)]
        ub = A.carve([4, D], F32, "ub")
        uTs = [A.carve([8, TT], BF16, "uT%d" % i) for i in range(2)]
        junk = A.carve([D], BF16, "junk")
        ss = A.carve([4], F32, "ss")
        sd = A.carve([4], F32, "sd")
        rstd = A.carve([4], F32, "rstd")
        rgst = [A.carve([6, TT], F32, "rgst%d" % i) for i in range(1)]
        nb = [2]

        def next_bank():
            b = banks[nb[0]]
            nb[0] = 2 + (nb[0] - 2 + 1) % 6
            return b

        def prologue(t):
            uT = uTs[t % 2]
            xt = xts[0]
            P.dma("sp", xt.ap, xsrc[t * TT:(t + 1) * TT, :].rearrange("(s p) d -> p s d", p=128),
                  reads=[xsrc_reg], writes=[xt.reg])
            for s in range(4):
                rmsnorm_rows(xt, s, ss, sd, rstd, junk, D)
            P.op("act", lambda e: e.activation(out=sd.ap, in_=ss.ap, func=AF.Sqrt, scale=1.0 / D, bias=EPS),
                 reads=[ss.reg], writes=[sd.reg])
            P.op("dve", lambda e: e.reciprocal(out=rstd.ap, in_=sd.ap), reads=[sd.reg], writes=[rstd.reg])
            for s in range(4):
                P.op("dve", lambda e, s=s, xt=xt: e.scalar_tensor_tensor(
                    out=ub.ap[:, s, :], in0=xt.ap[:, s, :], scalar=rstd.ap[:, s:s + 1], in1=g_rep.ap[:, 0, :],
                    op0=ALU.mult, op1=ALU.mult), reads=[xt.reg, rstd.reg, g_rep.reg], writes=[ub.reg])
            for c in range(8):
                bk = banks[c % 2]
                for s in range(4):
                    P.op("pe", lambda e, bk=bk, s=s, c=c: e.transpose(
                        out=bk.ap[:, s * 128:(s + 1) * 128], in_=ub.ap[:, s, c * 128:(c + 1) * 128],
                        identity=ident_f.ap), reads=[ub.reg, ident_f.reg], writes=[bk.reg])
                P.op("act", lambda e, bk=bk, c=c: e.activation(out=uT.ap[:, c, :], in_=bk.ap, func=AF.Copy),
                     reads=[bk.reg], writes=[uT.reg])
        prologue(0)
        for t in range(NT):
            uT = uTs[t % 2]
            for which, dst, scale in ((0, qT, 0.125), (1, kT, 1.0)):
                for j in range(4):
                    bk = next_bank()
                    col = which * 512 + j * 128
                    for c in range(8):
                        P.op("pe", lambda e, bk=bk, c=c, col=col, uT=uT: e.matmul(
                            bk.ap, lhsT=wsb.ap[:, c, col:col + 128], rhs=uT.ap[:, c, :],
                            start=(c == 0), stop=(c == 7)), reads=[wsb.reg, uT.reg], writes=[bk.reg])
                    P.op("act", lambda e, bk=bk, dst=dst, j=j, t=t, scale=scale: e.activation(
                        out=dst.ap[:, j, t * TT:(t + 1) * TT], in_=bk.ap, func=AF.Copy, scale=scale),
                        reads=[bk.reg], writes=[dst.reg])
            for s in range(4):
                bk = next_bank()
                for c in range(8):
                    P.op("pe", lambda e, bk=bk, c=c, s=s, uT=uT: e.matmul(
                        bk.ap, lhsT=uT.ap[:, c, s * 128:(s + 1) * 128], rhs=wsb.ap[:, c, 1024:1536],
                        start=(c == 0), stop=(c == 7)), reads=[wsb.reg, uT.reg], writes=[bk.reg])
                P.op("dve", lambda e, bk=bk, s=s, t=t: e.tensor_copy(
                    out=vaug.ap[:, t * 4 + s, :, 0:64], in_=bk.ap.rearrange("p (h d) -> p h d", h=8)),
                    reads=[bk.reg], writes=[vaug.reg])
            if t + 1 < NT:
                prologue(t + 1)
            rg = rgst[0]
            for j in range(6):
                bk = next_bank()
                col = 1536 + j * 128
                for c in range(8):
                    P.op("pe", lambda e, bk=bk, c=c, col=col, uT=uT: e.matmul(
                        bk.ap, lhsT=wsb.ap[:, c, col:col + 128], rhs=uT.ap[:, c, :],
                        start=(c == 0), stop=(c == 7)), reads=[wsb.reg, uT.reg], writes=[bk.reg])
                P.op("dve", lambda e, bk=bk, rg=rg, j=j: e.tensor_copy(out=rg.ap[:, j, :], in_=bk.ap),
                     reads=[bk.reg], writes=[rg.reg])
            P.dma("sp", rgsT.ap[:, t * TT:(t + 1) * TT].rearrange("(j p) n -> p j n", p=128), rg.ap,
                  reads=[rg.reg], writes=[rgsT.reg])
        return qT, kT, vaug, keep_top

    def phase_B(l, qT, kT, vaug, keep_top, fuse_c=True, casts=False):
        A.top = keep_top
        bias_sb = A.carve([8, 640], F32, "bias_sb")
        P.dma("sp", bias_sb.ap, biasT[l].rearrange("h k q -> k h q"), writes=[bias_sb.reg])
        gg_rep = A.carve([1, 512], F32, "gg_rep")
        P.dma("sp", gg_rep.ap, g_group[l:l + 1, 0:512].partition_broadcast(128), writes=[gg_rep.reg])
        if casts:
            issue_rest_casts(after=[bias_sb.reg, gg_rep.reg])
        stmp = [A.carve([640], F32, "stmp%d" % i) for i in range(2)]
        pT = [A.carve([640], BF16, "pT%d" % i) for i in range(2)]
        yatt = A.carve([8, 64], F32, "yatt")
        rc = A.carve([8], F32, "rc")
        junk = A.carve([512], BF16, "junkb")
        ss = A.carve([1], F32, "ssb")
        sd = A.carve([1], F32, "sdb")
        rstd = A.carve([1], F32, "rstdb")
        mxa = A.carve([512], F32, "mxa")
        mst = [A.carve([4, TT], BF16, "mst%d" % i) for i in range(2)]
        NIT = NS * 8
        stmp3 = stmp + [A.carve([640], F32, "stmp2")]
        pT3 = pT + [A.carve([640], BF16, "pT2")]

        def kts_of(qt):
            return [kt for kt in range(qt - 4, qt + 1) if kt >= 0]

        def emit_qk(idx):
            qt, h = divmod(idx, 8)
            kts = kts_of(qt)
            pb = (h % 2) * 64
            jp = h // 2
            sA, sB = banks[(idx % 2) * 2], banks[(idx % 2) * 2 + 1]
            for i, kt in enumerate(kts):
                dstb = sA if i < 4 else sB
                ci = i if i < 4 else 0
                P.op("pe", lambda e, dstb=dstb, ci=ci, kt=kt, pb=pb, jp=jp, qt=qt: e.matmul(
                    dstb.ap[:, ci * 128:(ci + 1) * 128],
                    lhsT=kT.ap[pb:pb + 64, jp, kt * 128:(kt + 1) * 128],
                    rhs=qT.ap[pb:pb + 64, jp, qt * 128:(qt + 1) * 128], start=True, stop=True),
                    reads=[kT.reg, qT.reg], writes=[dstb.reg])

        def emit_rest(idx):
            qt, h = divmod(idx, 8)
            kts = kts_of(qt)
            n = len(kts)
            sA, sB = banks[(idx % 2) * 2], banks[(idx % 2) * 2 + 1]
            st, pt_ = stmp3[idx % 3], pT3[idx % 3]
            j0 = kts[0] - qt + 4
            n4 = min(n, 4)
            P.op("dve", lambda e, st=st, sA=sA, h=h, j0=j0, n4=n4: e.tensor_tensor(
                out=st.ap[:, 0:n4 * 128], in0=sA.ap[:, 0:n4 * 128],
                in1=bias_sb.ap[:, h, j0 * 128:(j0 + n4) * 128], op=ALU.add),
                reads=[sA.reg, bias_sb.reg], writes=[st.reg])
            if n == 5:
                P.op("dve", lambda e, st=st, sB=sB, h=h: e.tensor_tensor(
                    out=st.ap[:, 512:640], in0=sB.ap[:, 0:128], in1=bias_sb.ap[:, h, 512:640], op=ALU.add),
                    reads=[sB.reg, bias_sb.reg], writes=[st.reg])
            P.op("act", lambda e, st=st, pt_=pt_, n=n: e.activation(
                out=pt_.ap[:, 0:n * 128], in_=st.ap[:, 0:n * 128], func=AF.Exp),
                reads=[st.reg], writes=[pt_.reg])
            ob = banks[4 + h // 4]
            hh = h % 4
            for i, kt in enumerate(kts):
                P.op("pe", lambda e, ob=ob, hh=hh, i=i, kt=kt, h=h, pt_=pt_, n=n: e.matmul(
                    ob.ap[:, hh * 65:(hh + 1) * 65], lhsT=pt_.ap[:, i * 128:(i + 1) * 128],
                    rhs=vaug.ap[:, kt, h, :], start=(i == 0), stop=(i == n - 1)),
                    reads=[pt_.reg, vaug.reg], writes=[ob.reg])

        cgen = gen_C(l) if fuse_c else iter(())
        emit_qk(0)
        for qt in range(NS):
            for h in range(8):
                idx = qt * 8 + h
                if idx + 1 < NIT:
                    emit_qk(idx + 1)
                emit_rest(idx)
                next(cgen, None)
            for half in range(2):
                ob = banks[4 + half]
                ov = ob.ap[:, 0:260].rearrange("p (h d) -> p h d", h=4)
                P.op("dve", lambda e, ov=ov, half=half: e.reciprocal(
                    out=rc.ap[:, half * 4:(half + 1) * 4], in_=ov[:, :, 64]), reads=[ob.reg], writes=[rc.reg])
                P.op("dve", lambda e, ov=ov, half=half: e.tensor_tensor(
                    out=yatt.ap[:, half * 4:(half + 1) * 4, :], in0=ov[:, :, 0:64],
                    in1=rc.ap[:, half * 4:(half + 1) * 4].unsqueeze(2).to_broadcast([128, 4, 64]), op=ALU.mult),
                    reads=[ob.reg, rc.reg], writes=[yatt.reg])
            yflat = yatt.ap.rearrange("p h d -> p (h d)")
            P.op("act", lambda e, yflat=yflat: e.activation(out=junk.ap, in_=yflat, func=AF.Square, accum_out=ss.ap),
                 reads=[yatt.reg], writes=[junk.reg, ss.reg])
            P.op("act", lambda e: e.activation(out=sd.ap, in_=ss.ap, func=AF.Ln, scale=1.0 / 512, bias=EPS),
                 reads=[ss.reg], writes=[sd.reg])
            P.op("act", lambda e: e.activation(out=rstd.ap, in_=sd.ap, func=AF.Exp, scale=-0.5), reads=[sd.reg], writes=[rstd.reg])
            P.op("dve", lambda e, yflat=yflat: e.scalar_tensor_tensor(
                out=mxa.ap, in0=yflat, scalar=rstd.ap[:, 0:1], in1=gg_rep.ap[:, 0, :], op0=ALU.mult, op1=ALU.mult),
                reads=[yatt.reg, rstd.reg, gg_rep.reg], writes=[mxa.reg])
            tb = banks[6]
            for c in range(4):
                P.op("pe", lambda e, c=c: e.transpose(out=tb.ap[:, c * 128:(c + 1) * 128],
                                                      in_=mxa.ap[:, c * 128:(c + 1) * 128], identity=ident_f.ap),
                     reads=[mxa.reg, ident_f.reg], writes=[tb.reg])
            ms = mst[(qt // 4) % 2]
            q4 = qt % 4
            P.op("act", lambda e, ms=ms, q4=q4: e.activation(
                out=ms.ap[:, :, q4 * 128:(q4 + 1) * 128], in_=tb.ap.rearrange("p (c t) -> p c t", c=4), func=AF.Copy),
                reads=[tb.reg], writes=[ms.reg])
            if q4 == 3:
                t = qt // 4
                P.dma("sp", mixedT.ap[0:512, t * TT:(t + 1) * TT].rearrange("(c p) n -> p c n", p=128), ms.ap,
                      reads=[ms.reg], writes=[mixedT.reg])
        for _ in cgen:
            pass

    def gen_C(l):
        CH = 1024
        NCH = L // CH
        cw, cb, bx, ba, lam, gg = (rgp[l][k] for k in ("cw", "cb", "bx", "ba", "lam", "gg"))
        sp_ = A.carve([2], F32, "sp_")
        c8 = A.carve([2], F32, "c8")
        c16 = A.carve([2], F32, "c16")
        P.op("act", lambda e: e.activation(out=sp_.ap, in_=lam.ap, func=AF.Exp, scale=-1.0), reads=[lam.reg], writes=[sp_.reg])
        P.op("act", lambda e: e.activation(out=sp_.ap, in_=sp_.ap, func=AF.Ln, bias=1.0), reads=[sp_.reg], writes=[sp_.reg])
        P.op("dve", lambda e: e.tensor_scalar(out=c8.ap, in0=sp_.ap, scalar1=-8.0, scalar2=None, op0=ALU.mult),
             reads=[sp_.reg], writes=[c8.reg])
        P.op("dve", lambda e: e.tensor_scalar(out=c16.ap, in0=sp_.ap, scalar1=-16.0, scalar2=None, op0=ALU.mult),
             reads=[sp_.reg], writes=[c16.reg])
        wf = A.carve([2, 2, 128], F32, "wf_bd")
        wb = A.carve([2, 2, 128], BF16, "wb_bd")
        P.op("dve", lambda e: e.memset(wf.ap, 0.0), writes=[wf.reg])
        for gi, src in enumerate((rg_wx, rg_wa)):
            for ct in range(2):
                for blk in range(2):
                    P.dma("sp", wf.ap[blk * 64:(blk + 1) * 64, gi, ct, blk * 64:(blk + 1) * 64], src[l, 2 * ct + blk],
                          writes=[wf.reg])
        P.op("dve", lambda e: e.tensor_copy(out=wb.ap, in_=wf.ap), reads=[wf.reg], writes=[wb.reg])
        yield
        xr = A.carve([CH + 3], F32, "xr")
        gate = A.carve([CH], F32, "gate")
        xc = A.carve([CH], F32, "xc")
        xcb = A.carve([CH], BF16, "xcb")
        gx = A.carve([CH], F32, "gx")
        ga = A.carve([CH], F32, "ga")
        aa = A.carve([CH], F32, "aa")
        hh = A.carve([CH], F32, "hh")
        ys = [A.carve([CH], F32, "yrg%d" % i) for i in range(2)]
        sq = A.carve([TT], F32, "sqrg")
        sdc = A.carve([TT], F32, "sdc")
        rstdc = A.carve([TT], F32, "rstdc")
        mo = A.carve([2, TT], BF16, "morg")
        hcar = A.carve([2], F32, "hcar")
        P.op("dve", lambda e: e.memset(hcar.ap, 0.0), writes=[hcar.reg])
        bk = banks[7]
        for ch in range(NCH):
            t0 = ch * CH
            for ct in range(2):
                r0 = ct * 128
                if ch == 0:
                    P.op("dve", lambda e: e.memset(xr.ap[:, 0:3], 0.0), writes=[xr.reg])
                    P.dma("sp", xr.ap[:, 3:CH + 3], rgsT.ap[r0:r0 + 128, 0:CH], reads=[rgsT.reg], writes=[xr.reg])
                else:
                    P.dma("sp", xr.ap, rgsT.ap[r0:r0 + 128, t0 - 3:t0 + CH], reads=[rgsT.reg], writes=[xr.reg])
                P.dma("sp", gate.ap, rgsT.ap[256 + r0:256 + r0 + 128, t0:t0 + CH], reads=[rgsT.reg], writes=[gate.reg])
                P.op("dve", lambda e, ct=ct: e.tensor_scalar(out=xc.ap, in0=xr.ap[:, 3:CH + 3], scalar1=cw.ap[:, ct, 3:4],
                                                             scalar2=cb.ap[:, ct:ct + 1], op0=ALU.mult, op1=ALU.add),
                     reads=[xr.reg, cw.reg, cb.reg], writes=[xc.reg])
                yield
                for k in range(1, 4):
                    P.op("dve", lambda e, ct=ct, k=k: e.scalar_tensor_tensor(
                        out=xc.ap, in0=xr.ap[:, 3 - k:CH + 3 - k], scalar=cw.ap[:, ct, 3 - k:4 - k], in1=xc.ap,
                        op0=ALU.mult, op1=ALU.add), reads=[xr.reg, cw.reg, xc.reg], writes=[xc.reg])
                    yield
                P.op("dve", lambda e: e.tensor_copy(out=xcb.ap, in_=xc.ap), reads=[xc.reg], writes=[xcb.reg])
                yield
                for gi, (bvec, dst) in enumerate(((bx, gx), (ba, ga))):
                    for piece in range(CH // TT):
                        sl = slice(piece * TT, (piece + 1) * TT)
                        P.op("pe", lambda e, gi=gi, ct=ct, sl=sl: e.matmul(
                            bk.ap, lhsT=wb.ap[:, gi, ct, :], rhs=xcb.ap[:, sl], start=True, stop=True),
                            reads=[wb.reg, xcb.reg], writes=[bk.reg])
                        P.op("act", lambda e, bvec=bvec, dst=dst, ct=ct, sl=sl: e.activation(
                            out=dst.ap[:, sl], in_=bk.ap, func=AF.Sigmoid, bias=bvec.ap[:, ct:ct + 1]),
                            reads=[bk.reg, bvec.reg], writes=[dst.reg])
                yield
                P.op("act", lambda e, ct=ct: e.activation(out=aa.ap, in_=ga.ap, func=AF.Exp, scale=c8.ap[:, ct:ct + 1]),
                     reads=[ga.reg, c8.reg], writes=[aa.reg])
                P.op("act", lambda e, ct=ct: e.activation(out=ga.ap, in_=ga.ap, func=AF.Exp, scale=c16.ap[:, ct:ct + 1]),
                     reads=[ga.reg, c16.reg], writes=[ga.reg])
                yield
                P.op("act", lambda e: e.activation(out=ga.ap, in_=ga.ap, func=AF.Ln, scale=-1.0, bias=1.0),
                     reads=[ga.reg], writes=[ga.reg])
                P.op("act", lambda e: e.activation(out=ga.ap, in_=ga.ap, func=AF.Exp, scale=0.5),
                     reads=[ga.reg], writes=[ga.reg])
                yield
                P.op("dve", lambda e: e.tensor_tensor(out=gx.ap, in0=gx.ap, in1=ga.ap, op=ALU.mult),
                     reads=[gx.reg, ga.reg], writes=[gx.reg])
                yield
                P.op("dve", lambda e: e.tensor_tensor(out=gx.ap, in0=gx.ap, in1=xc.ap, op=ALU.mult),
                     reads=[gx.reg, xc.reg], writes=[gx.reg])
                yield
                P.op("dve", lambda e, ct=ct: e.tensor_tensor_scan(out=hh.ap, data0=aa.ap, data1=gx.ap, initial=hcar.ap[:, ct:ct + 1],
                                                                  op0=ALU.mult, op1=ALU.add),
                     reads=[aa.reg, gx.reg, hcar.reg], writes=[hh.reg])
                P.op("act", lambda e: e.activation(out=gate.ap, in_=gate.ap, func=AF.Gelu_apprx_tanh),
                     reads=[gate.reg], writes=[gate.reg])
                yield
                P.op("dve", lambda e, ct=ct: e.tensor_copy(out=hcar.ap[:, ct:ct + 1], in_=hh.ap[:, CH - 1:CH]),
                     reads=[hh.reg], writes=[hcar.reg])
                y = ys[ct]
                P.op("dve", lambda e, y=y: e.tensor_tensor(out=y.ap, in0=hh.ap, in1=gate.ap, op=ALU.mult),
                     reads=[hh.reg, gate.reg], writes=[y.reg])
                yield
            for piece in range(CH // TT):
                sl = slice(piece * TT, (piece + 1) * TT)
                gsl = slice(t0 + piece * TT, t0 + (piece + 1) * TT)
                for ct in range(2):
                    P.op("dve", lambda e, ct=ct, sl=sl: e.tensor_tensor(out=sq.ap, in0=ys[ct].ap[:, sl], in1=ys[ct].ap[:, sl], op=ALU.mult),
                         reads=[ys[ct].reg], writes=[sq.reg])
                    P.op("pe", lambda e, ct=ct: e.matmul(bk.ap, lhsT=ones_f.ap, rhs=sq.ap, start=(ct == 0), stop=(ct == 1)),
                         reads=[ones_f.reg, sq.reg], writes=[bk.reg])
                    yield
                P.op("act", lambda e: e.activation(out=sdc.ap, in_=bk.ap, func=AF.Ln, scale=1.0 / 256, bias=EPS),
                     reads=[bk.reg], writes=[sdc.reg])
                P.op("act", lambda e: e.activation(out=rstdc.ap, in_=sdc.ap, func=AF.Exp, scale=-0.5), reads=[sdc.reg], writes=[rstdc.reg])
                yield
                for ct in range(2):
                    P.op("dve", lambda e, ct=ct, sl=sl: e.scalar_tensor_tensor(
                        out=mo.ap[:, ct, :], in0=ys[ct].ap[:, sl], scalar=gg.ap[:, ct:ct + 1], in1=rstdc.ap,
                        op0=ALU.mult, op1=ALU.mult), reads=[ys[ct].reg, gg.reg, rstdc.reg], writes=[mo.reg])
                P.dma("sp", mixedT.ap[512:768, gsl].rearrange("(c p) n -> p c n", p=128), mo.ap,
                      reads=[mo.reg], writes=[mixedT.reg])
                yield

    def phase_D(l):
        A.top = A_BASE
        G = 16
        TWO_PI = 6.283185307179586
        C1 = 6.28125
        C2 = TWO_PI - C1

        def small(name, free=(G,)):
            return A.carve(list(free), F32, name)

        are, aim, ldt = small("are"), small("aim"), small("ldt")
        for half in range(2):
            P.dma("sp", are.ap[half * 64:(half + 1) * 64, :], s5_a_re[l].rearrange("g p -> p g"), writes=[are.reg],
                  allow_slow_non_contiguous=True)
            P.dma("sp", aim.ap[half * 64:(half + 1) * 64, :], s5_a_im[l].rearrange("g p -> p g"), writes=[aim.reg],
                  allow_slow_non_contiguous=True)
        P.dma("sp", ldt.ap.rearrange("p (o g) -> p o g", o=1), s5_log_dt[l:l + 1, :].partition_broadcast(128),
              writes=[ldt.reg])
        dvec = vec2(s5_d[l], "dvec")
        gg = vec2(g_group[l, 768:1024], "ggs5")
        dt_ = small("dt_")
        rr = small("rr")
        th = small("th")
        kk = small("kk")
        tmp = small("tmp")
        tmp2 = small("tmp2")
        cs = [small("cs_c"), small("cs_s")]
        cs2 = [small("cs2_c"), small("cs2_s")]

        def dv(fn, reads, writes):
            P.op("dve", fn, reads=[r.reg for r in reads], writes=[w.reg for w in writes])

        P.op("act", lambda e: e.activation(out=dt_.ap, in_=ldt.ap, func=AF.Exp), reads=[ldt.reg], writes=[dt_.reg])
        dv(lambda e: e.tensor_tensor(out=tmp.ap, in0=are.ap, in1=dt_.ap, op=ALU.mult), [are, dt_], [tmp])
        P.op("act", lambda e: e.activation(out=rr.ap, in_=tmp.ap, func=AF.Exp), reads=[tmp.reg], writes=[rr.reg])
        dv(lambda e: e.tensor_tensor(out=th.ap, in0=aim.ap, in1=dt_.ap, op=ALU.mult), [aim, dt_], [th])
        dv(lambda e: e.tensor_scalar(out=kk.ap, in0=th.ap, scalar1=3.141592653589793, scalar2=None, op0=ALU.is_gt), [th], [kk])
        for j in range(1, 5):
            dv(lambda e, j=j: e.tensor_scalar(out=tmp.ap, in0=th.ap, scalar1=(2 * j + 1) * 3.141592653589793, scalar2=None,
                                              op0=ALU.is_gt), [th], [tmp])
            dv(lambda e: e.tensor_tensor(out=kk.ap, in0=kk.ap, in1=tmp.ap, op=ALU.add), [kk, tmp], [kk])
        thr = small("thr")
        dv(lambda e: e.scalar_tensor_tensor(out=thr.ap, in0=kk.ap, scalar=-C1, in1=th.ap, op0=ALU.mult, op1=ALU.add), [kk, th], [thr])
        dv(lambda e: e.scalar_tensor_tensor(out=thr.ap, in0=kk.ap, scalar=-C2, in1=thr.ap, op0=ALU.mult, op1=ALU.add), [kk, thr], [thr])
        P.op("act", lambda e: e.activation(out=cs[1].ap, in_=thr.ap, func=AF.Sin), reads=[thr.reg], writes=[cs[1].reg])
        thc = small("thc")
        dv(lambda e: e.tensor_scalar(out=tmp.ap, in0=thr.ap, scalar1=3.141592653589793 / 2, scalar2=None, op0=ALU.is_gt), [thr], [tmp])
        dv(lambda e: e.scalar_tensor_tensor(out=thc.ap, in0=tmp.ap, scalar=-C1, in1=thr.ap, op0=ALU.mult, op1=ALU.add), [tmp, thr], [thc])
        dv(lambda e: e.scalar_tensor_tensor(out=thc.ap, in0=tmp.ap, scalar=-C2, in1=thc.ap, op0=ALU.mult, op1=ALU.add), [tmp, thc], [thc])
        P.op("act", lambda e: e.activation(out=cs[0].ap, in_=thc.ap, func=AF.Sin, bias=3.141592653589793 / 2),
             reads=[thc.reg], writes=[cs[0].reg])
        nr, ni, den, cre, cim = small("nr"), small("ni"), small("den"), small("cre"), small("cim")
        dv(lambda e: e.tensor_tensor(out=nr.ap, in0=rr.ap, in1=cs[0].ap, op=ALU.mult), [rr, cs[0]], [nr])
        dv(lambda e: e.tensor_scalar(out=nr.ap, in0=nr.ap, scalar1=-1.0, scalar2=None, op0=ALU.add), [nr], [nr])
        dv(lambda e: e.tensor_tensor(out=ni.ap, in0=rr.ap, in1=cs[1].ap, op=ALU.mult), [rr, cs[1]], [ni])
        dv(lambda e: e.tensor_tensor(out=den.ap, in0=are.ap, in1=are.ap, op=ALU.mult), [are], [den])
        dv(lambda e: e.tensor_tensor(out=tmp.ap, in0=aim.ap, in1=aim.ap, op=ALU.mult), [aim], [tmp])
        dv(lambda e: e.tensor_tensor(out=den.ap, in0=den.ap, in1=tmp.ap, op=ALU.add), [den, tmp], [den])
        dv(lambda e: e.reciprocal(out=den.ap, in_=den.ap), [den], [den])
        dv(lambda e: e.tensor_tensor(out=cre.ap, in0=nr.ap, in1=are.ap, op=ALU.mult), [nr, are], [cre])
        dv(lambda e: e.tensor_tensor(out=tmp.ap, in0=ni.ap, in1=aim.ap, op=ALU.mult), [ni, aim], [tmp])
        dv(lambda e: e.tensor_tensor(out=cre.ap, in0=cre.ap, in1=tmp.ap, op=ALU.add), [cre, tmp], [cre])
        dv(lambda e: e.tensor_tensor(out=cre.ap, in0=cre.ap, in1=den.ap, op=ALU.mult), [cre, den], [cre])
        dv(lambda e: e.tensor_tensor(out=cim.ap, in0=ni.ap, in1=are.ap, op=ALU.mult), [ni, are], [cim])
        dv(lambda e: e.tensor_tensor(out=tmp.ap, in0=nr.ap, in1=aim.ap, op=ALU.mult), [nr, aim], [tmp])
        dv(lambda e: e.tensor_tensor(out=cim.ap, in0=cim.ap, in1=tmp.ap, op=ALU.subtract), [cim, tmp], [cim])
        dv(lambda e: e.tensor_tensor(out=cim.ap, in0=cim.ap, in1=den.ap, op=ALU.mult), [cim, den], [cim])
        bre = A.carve([G, 16], F32, "bre")
        bim = A.carve([G, 16], F32, "bim")
        for half in range(2):
            P.dma("sp", bre.ap[half * 64:(half + 1) * 64], s5_b_re[l].rearrange("g p c -> p g c"), writes=[bre.reg])
            P.dma("sp", bim.ap[half * 64:(half + 1) * 64], s5_b_im[l].rearrange("g p c -> p g c"), writes=[bim.reg])
        ta, tb_, tc_, td = [A.carve([G, 16], F32, "t%s" % n) for n in "abcd"]
        X1 = A.carve([G, 16], F32, "X1")
        X2 = A.carve([G, 16], F32, "X2")

        def bc(t, m):
            return t.ap.unsqueeze(2).to_broadcast([128, G, m])

        dv(lambda e: e.tensor_tensor(out=ta.ap, in0=bre.ap, in1=bc(cre, 16), op=ALU.mult), [bre, cre], [ta])
        dv(lambda e: e.tensor_tensor(out=tb_.ap, in0=bim.ap, in1=bc(cim, 16), op=ALU.mult), [bim, cim], [tb_])
        dv(lambda e: e.tensor_tensor(out=tc_.ap, in0=bim.ap, in1=bc(cre, 16), op=ALU.mult), [bim, cre], [tc_])
        dv(lambda e: e.tensor_tensor(out=td.ap, in0=bre.ap, in1=bc(cim, 16), op=ALU.mult), [bre, cim], [td])
        lo, hi = slice(0, 64), slice(64, 128)
        dv(lambda e: e.tensor_tensor(out=X1.ap[lo], in0=ta.ap[lo], in1=tb_.ap[lo], op=ALU.subtract), [ta, tb_], [X1])
        dv(lambda e: e.tensor_tensor(out=X2.ap[lo], in0=tc_.ap[lo], in1=td.ap[lo], op=ALU.add), [tc_, td], [X2])
        dv(lambda e: e.tensor_tensor(out=X1.ap[hi], in0=tc_.ap[hi], in1=td.ap[hi], op=ALU.add), [tc_, td], [X1])
        dv(lambda e: e.tensor_tensor(out=X2.ap[hi], in0=tb_.ap[hi], in1=ta.ap[hi], op=ALU.subtract), [ta, tb_], [X2])
        Zp = [A.carve([G, 128], F32, "Zp%d" % i) for i in range(2)]
        Win = [A.carve([G, 128], BF16, "Win%d" % i) for i in range(2)]
        for i, X in enumerate((X1, X2)):
            P.op("pool", lambda e, i=i: e.memset(Zp[i].ap, 0.0), writes=[Zp[i].reg])
            for g in range(G):
                s0 = (g % 8) * 16
                P.op("dve", lambda e, i=i, g=g, s0=s0, X=X: e.tensor_copy(out=Zp[i].ap[:, g, s0:s0 + 16], in_=X.ap[:, g, :]),
                     reads=[X.reg], writes=[Zp[i].reg])
            for g in range(G):
                bk = banks[g % 2]
                P.op("pe", lambda e, bk=bk, i=i, g=g: e.transpose(out=bk.ap[:, 0:128], in_=Zp[i].ap[:, g, :], identity=ident_f.ap),
                     reads=[Zp[i].reg, ident_f.reg], writes=[bk.reg])
                P.op("act", lambda e, bk=bk, i=i, g=g: e.activation(out=Win[i].ap[:, g, :], in_=bk.ap[:, 0:128], func=AF.Copy),
                     reads=[bk.reg], writes=[Win[i].reg])
        cnat = [A.carve([2, 128], F32, "cnat%d" % i) for i in range(2)]
        CT = [A.carve([G, 16], F32, "CT%d" % i) for i in range(2)]
        for i, src in enumerate((s5_c_re, s5_c_im)):
            v = src[l].rearrange("g c p -> (g c) p").rearrange("(t r) p -> r t p", r=128)
            for dup in range(2):
                P.dma("sp", cnat[i].ap[:, :, dup * 64:(dup + 1) * 64], v, writes=[cnat[i].reg])
            for t in range(2):
                bk = banks[2 + t]
                P.op("pe", lambda e, bk=bk, i=i, t=t: e.transpose(out=bk.ap[:, 0:128], in_=cnat[i].ap[:, t, :], identity=ident_f.ap),
                     reads=[cnat[i].reg, ident_f.reg], writes=[bk.reg])
                P.op("act", lambda e, bk=bk, i=i, t=t: e.activation(
                    out=CT[i].ap[:, t * 8:(t + 1) * 8, :], in_=bk.ap[:, 0:128].rearrange("p (g c) -> p g c", g=8), func=AF.Copy),
                    reads=[bk.reg], writes=[CT[i].reg])
        Wo = [A.carve([G, 128], BF16, "Wo%d" % i) for i in range(2)]
        for i in range(2):
            P.op("pool", lambda e, i=i: e.memset(Wo[i].ap, 0.0), writes=[Wo[i].reg])
        for g in range(G):
            s0 = (g % 8) * 16
            P.op("dve", lambda e, g=g, s0=s0: e.tensor_copy(out=Wo[0].ap[lo, g, s0:s0 + 16], in_=CT[0].ap[lo, g, :]),
                 reads=[CT[0].reg], writes=[Wo[0].reg])
            P.op("dve", lambda e, g=g, s0=s0: e.tensor_scalar(out=Wo[0].ap[hi, g, s0:s0 + 16], in0=CT[1].ap[hi, g, :],
                                                               scalar1=-1.0, scalar2=None, op0=ALU.mult),
                 reads=[CT[1].reg], writes=[Wo[0].reg])
            P.op("dve", lambda e, g=g, s0=s0: e.tensor_scalar(out=Wo[1].ap[lo, g, s0:s0 + 16], in0=CT[1].ap[lo, g, :],
                                                               scalar1=-1.0, scalar2=None, op0=ALU.mult),
                 reads=[CT[1].reg], writes=[Wo[1].reg])
            P.op("dve", lambda e, g=g, s0=s0: e.tensor_scalar(out=Wo[1].ap[hi, g, s0:s0 + 16], in0=CT[0].ap[hi, g, :],
                                                               scalar1=-1.0, scalar2=None, op0=ALU.mult),
                 reads=[CT[0].reg], writes=[Wo[1].reg])
        cosT = A.carve([G, TT], F32, "cosT")
        sinT = A.carve([G, TT], F32, "sinT")
        P.op("pool", lambda e: e.memset(cosT.ap[:, :, 0:1], 1.0), writes=[cosT.reg])
        P.op("pool", lambda e: e.memset(sinT.ap[:, :, 0:1], 0.0), writes=[sinT.reg])
        w1 = A.carve([G, 128], F32, "w1")
        w2 = A.carve([G, 128], F32, "w2")
        cur, nxt = cs, cs2
        m = 1
        while m < TT:
            cj, sj = cur
            for off in range(0, m, 128):
                bl = min(m, 128)
                a0 = slice(off, off + bl)
                a1 = slice(m + off, m + off + bl)
                dv(lambda e, cj=cj, bl=bl, a0=a0: e.tensor_tensor(out=w1.ap[:, :, 0:bl], in0=cosT.ap[:, :, a0], in1=bc(cj, bl), op=ALU.mult), [cosT, cj], [w1])
                dv(lambda e, sj=sj, bl=bl, a0=a0: e.tensor_tensor(out=w2.ap[:, :, 0:bl], in0=sinT.ap[:, :, a0], in1=bc(sj, bl), op=ALU.mult), [sinT, sj], [w2])
                dv(lambda e, bl=bl, a1=a1: e.tensor_tensor(out=cosT.ap[:, :, a1], in0=w1.ap[:, :, 0:bl], in1=w2.ap[:, :, 0:bl], op=ALU.subtract), [w1, w2], [cosT])
                dv(lambda e, cj=cj, bl=bl, a0=a0: e.tensor_tensor(out=w1.ap[:, :, 0:bl], in0=sinT.ap[:, :, a0], in1=bc(cj, bl), op=ALU.mult), [sinT, cj], [w1])
                dv(lambda e, sj=sj, bl=bl, a0=a0: e.tensor_tensor(out=w2.ap[:, :, 0:bl], in0=cosT.ap[:, :, a0], in1=bc(sj, bl), op=ALU.mult), [cosT, sj], [w2])
                dv(lambda e, bl=bl, a1=a1: e.tensor_tensor(out=sinT.ap[:, :, a1], in0=w1.ap[:, :, 0:bl], in1=w2.ap[:, :, 0:bl], op=ALU.add), [w1, w2], [sinT])
            nc_, ns_ = nxt
            dv(lambda e, cj=cj: e.tensor_tensor(out=tmp.ap, in0=cj.ap, in1=cj.ap, op=ALU.mult), [cj], [tmp])
            dv(lambda e, sj=sj: e.tensor_tensor(out=tmp2.ap, in0=sj.ap, in1=sj.ap, op=ALU.mult), [sj], [tmp2])
            dv(lambda e, nc_=nc_: e.tensor_tensor(out=nc_.ap, in0=tmp.ap, in1=tmp2.ap, op=ALU.subtract), [tmp, tmp2], [nc_])
            dv(lambda e, ns_=ns_, cj=cj, sj=sj: e.scalar_tensor_tensor(out=ns_.ap, in0=cj.ap, scalar=2.0, in1=sj.ap, op0=ALU.mult, op1=ALU.mult), [cj, sj], [ns_])
            cur, nxt = nxt, cur
            m *= 2
        c9, s9 = cur
        ns9 = small("ns9")
        dv(lambda e: e.tensor_scalar(out=ns9.ap, in0=s9.ap, scalar1=-1.0, scalar2=None, op0=ALU.mult), [s9], [ns9])
        Mr = A.carve([G, 128], F32, "Mr")
        for (ph, csl, src, idsl) in ((slice(0, 128), slice(0, 128), c9, slice(0, 128)),
                                     (lo, slice(64, 128), s9, slice(0, 64)),
                                     (hi, slice(0, 64), ns9, slice(64, 128))):
            npart = ph.stop - ph.start
            ncol = csl.stop - csl.start
            P.op("dve", lambda e, ph=ph, csl=csl, src=src, idsl=idsl, npart=npart, ncol=ncol: e.tensor_tensor(
                out=Mr.ap[ph, :, csl],
                in0=ident_f.ap[ph, idsl].unsqueeze(1).to_broadcast([npart, G, ncol]),
                in1=src.ap[ph, :].unsqueeze(2).to_broadcast([npart, G, ncol]), op=ALU.mult),
                reads=[ident_f.reg, src.reg], writes=[Mr.reg])
        wglu = A.carve([2, 256], F32, "wglu")
        P.dma("sp", wglu.ap, s5_w_glu[l].rearrange("(ct p) j -> p ct j", p=128), writes=[wglu.reg])
        carry = small("carry")
        P.op("pool", lambda e: e.memset(carry.ap, 0.0), writes=[carry.reg])
        creg = [carry.reg.sub(g) for g in range(G)]
        NB = 3
        usf = [A.carve([2, TT], F32, "usf%d" % i) for i in range(2)]
        usb = [A.carve([2, TT], BF16, "usb%d" % i) for i in range(2)]
        t1 = [A.carve([TT], F32, "t1_%d" % i) for i in range(NB)]
        t2 = [A.carve([TT], F32, "t2_%d" % i) for i in range(NB)]
        qin = [A.carve([TT], F32, "qin%d" % i) for i in range(NB)]
        qq = [A.carve([TT], F32, "qq%d" % i) for i in range(NB)]
        Z1 = [A.carve([TT], BF16, "Z1_%d" % i) for i in range(NB)]
        Z2 = [A.carve([TT], BF16, "Z2_%d" % i) for i in range(NB)]
        y0 = A.carve([TT], F32, "y0")
        y1 = [A.carve([TT], F32, "y1_%d" % i) for i in range(2)]
        y2 = [A.carve([TT], F32, "y2_%d" % i) for i in range(2)]
        sq = [A.carve([TT], F32, "sqs5_%d" % i) for i in range(2)]
        sig = A.carve([TT], F32, "sig")
        sdc = A.carve([TT], F32, "sdc5")
        rstdc = A.carve([TT], F32, "rstdc5")
        mo = A.carve([2, TT], BF16, "mos5")
        N_IT = NT * 16
        deferred = {}

        def later(step, fn):
            deferred.setdefault(step, []).append(fn)

        def info(i):
            ch, r = divmod(i, 16)
            ct, gl = divmod(r, 8)
            return ch, ct, gl, ct * 8 + gl

        def load_chunk(ch):
            sl = slice(ch * TT, (ch + 1) * TT)
            uf, ub_ = usf[ch % 2], usb[ch % 2]
            P.dma("sp", uf.ap, rgsT.ap[512:768, sl].rearrange("(ct p) n -> p ct n", p=128), reads=[rgsT.reg], writes=[uf.reg])
            P.op("act", lambda e, uf=uf, ub_=ub_: e.activation(out=ub_.ap, in_=uf.ap, func=AF.Copy), reads=[uf.reg], writes=[ub_.reg])

        def S1(i):
            ch, ct, gl, g = info(i)
            ub_ = usb[ch % 2]
            p1, p2 = banks[(i % 2) * 2], banks[(i % 2) * 2 + 1]
            P.op("pe", lambda e: e.matmul(p1.ap, lhsT=Win[0].ap[:, g, :], rhs=ub_.ap[:, ct, :], start=True, stop=True),
                 reads=[Win[0].reg, ub_.reg], writes=[p1.reg])
            P.op("pe", lambda e: e.matmul(p2.ap, lhsT=Win[1].ap[:, g, :], rhs=ub_.ap[:, ct, :], start=True, stop=True),
                 reads=[Win[1].reg, ub_.reg], writes=[p2.reg])

        def S23(i):
            ch, ct, gl, g = info(i)
            b = i % NB
            p1, p2 = banks[(i % 2) * 2], banks[(i % 2) * 2 + 1]
            P.op("dve", lambda e: e.tensor_tensor(out=t1[b].ap, in0=p1.ap, in1=cosT.ap[:, g, :], op=ALU.mult),
                 reads=[p1.reg, cosT.reg], writes=[t1[b].reg])
            P.op("dve", lambda e: e.tensor_tensor(out=t2[b].ap, in0=p2.ap, in1=sinT.ap[:, g, :], op=ALU.mult),
                 reads=[p2.reg, sinT.reg], writes=[t2[b].reg])
            P.op("pool", lambda e: e.tensor_tensor(out=qin[b].ap, in0=t1[b].ap, in1=t2[b].ap, op=ALU.add),
                 reads=[t1[b].reg, t2[b].reg], writes=[qin[b].reg])

        def S456(i):
            ch, ct, gl, g = info(i)
            b = i % NB
            P.op("dve", lambda e: e.tensor_tensor_scan(
                out=qq[b].ap, data0=rr.ap[:, g:g + 1].to_broadcast([128, TT]), data1=qin[b].ap,
                initial=carry.ap[:, g:g + 1], op0=ALU.mult, op1=ALU.add),
                reads=[rr.reg, qin[b].reg, creg[g]], writes=[qq[b].reg])
            if ch < NT - 1:
                cbk = banks[6 + (i % 2)]
                P.op("pe", lambda e: e.matmul(cbk.ap[:, g:g + 1], lhsT=Mr.ap[:, g, :], rhs=qq[b].ap[:, TT - 1:TT], start=True, stop=True),
                     reads=[Mr.reg, qq[b].reg], writes=[cbk.reg])
                P.op("act", lambda e: e.activation(out=carry.ap[:, g:g + 1], in_=cbk.ap[:, g:g + 1], func=AF.Copy),
                     reads=[cbk.reg], writes=[creg[g]])
            P.op("dve", lambda e: e.tensor_tensor(out=Z1[b].ap, in0=qq[b].ap, in1=cosT.ap[:, g, :], op=ALU.mult),
                 reads=[qq[b].reg, cosT.reg], writes=[Z1[b].reg])
            P.op("pool", lambda e: e.tensor_tensor(out=Z2[b].ap, in0=qq[b].ap, in1=sinT.ap[:, g, :], op=ALU.mult),
                 reads=[qq[b].reg, sinT.reg], writes=[Z2[b].reg])

        def S7(i, k):
            ch, ct, gl, g = info(i)
            b = i % NB
            yb = banks[4 + ct]
            P.op("pe", lambda e: e.matmul(yb.ap, lhsT=Wo[0].ap[:, g, :], rhs=Z1[b].ap, start=(gl == 0), stop=False),
                 reads=[Wo[0].reg, Z1[b].reg], writes=[yb.reg])
            P.op("pe", lambda e: e.matmul(yb.ap, lhsT=Wo[1].ap[:, g, :], rhs=Z2[b].ap, start=False, stop=(gl == 7)),
                 reads=[Wo[1].reg, Z2[b].reg], writes=[yb.reg])
            if gl == 7:
                uf = usf[ch % 2]
                later(k + 1, lambda: post_ct(ch, ct, yb, uf))
                if ct == 1:
                    later(k + 2, lambda: post_a(ch))
                    later(k + 3, lambda: post_b(ch))
                    later(k + 4, lambda: post_c(ch))

        def post_ct(ch, ct, yb, uf):
            P.op("dve", lambda e: e.scalar_tensor_tensor(
                out=y0.ap, in0=uf.ap[:, ct, :], scalar=dvec.ap[:, ct:ct + 1], in1=yb.ap, op0=ALU.mult, op1=ALU.add),
                reads=[uf.reg, dvec.reg, yb.reg], writes=[y0.reg])
            P.op("act", lambda e: e.activation(out=y1[ct].ap, in_=y0.ap, func=AF.Gelu_apprx_tanh),
                 reads=[y0.reg], writes=[y1[ct].reg])

        def post_a(ch):
            for co in range(2):
                zb = banks[7]
                for ct in range(2):
                    P.op("pe", lambda e, ct=ct, co=co: e.matmul(zb.ap, lhsT=wglu.ap[:, ct, co * 128:(co + 1) * 128], rhs=y1[ct].ap,
                                                                start=(ct == 0), stop=(ct == 1)),
                         reads=[wglu.reg, y1[ct].reg], writes=[zb.reg])
                P.op("act", lambda e, co=co: e.activation(out=sig.ap if co == 0 else sdc.ap, in_=zb.ap, func=AF.Sigmoid),
                     reads=[zb.reg], writes=[sig.reg if co == 0 else sdc.reg])

        def post_b(ch):
            for co in range(2):
                sg = sig if co == 0 else sdc
                P.op("dve", lambda e, co=co, sg=sg: e.tensor_tensor(out=y2[co].ap, in0=y1[co].ap, in1=sg.ap, op=ALU.mult),
                     reads=[y1[co].reg, sg.reg], writes=[y2[co].reg])
                P.op("act", lambda e, co=co: e.activation(out=sq[co].ap, in_=y2[co].ap, func=AF.Square),
                     reads=[y2[co].reg], writes=[sq[co].reg])
            vb = banks[7]
            for co in range(2):
                P.op("pe", lambda e, co=co: e.matmul(vb.ap, lhsT=ones_f.ap, rhs=sq[co].ap, start=(co == 0), stop=(co == 1)),
                     reads=[ones_f.reg, sq[co].reg], writes=[vb.reg])
            P.op("act", lambda e: e.activation(out=sdc.ap, in_=vb.ap, func=AF.Sqrt, scale=1.0 / 256, bias=EPS),
                 reads=[vb.reg], writes=[sdc.reg])

        def post_c(ch):
            sl = slice(ch * TT, (ch + 1) * TT)
            P.op("dve", lambda e: e.reciprocal(out=rstdc.ap, in_=sdc.ap), reads=[sdc.reg], writes=[rstdc.reg])
            for co in range(2):
                P.op("dve", lambda e, co=co: e.scalar_tensor_tensor(
                    out=mo.ap[:, co, :], in0=y2[co].ap, scalar=gg.ap[:, co:co + 1], in1=rstdc.ap, op0=ALU.mult, op1=ALU.mult),
                    reads=[y2[co].reg, gg.reg, rstdc.reg], writes=[mo.reg])
            P.dma("sp", mixedT.ap[768:1024, sl].rearrange("(c p) n -> p c n", p=128), mo.ap, reads=[mo.reg], writes=[mixedT.reg])

        load_chunk(0)
        for k in range(N_IT + 8):
            if k % 4 == 1:
                pop_cast()
            if k < N_IT:
                if k % 16 == 8 and k // 16 + 1 < NT:
                    load_chunk(k // 16 + 1)
                S1(k)
            if 0 <= k - 1 < N_IT:
                S23(k - 1)
            if 0 <= k - 2 < N_IT:
                S456(k - 2)
            if 0 <= k - 3 < N_IT:
                S7(k - 3, k)
            for fn in deferred.pop(k, []):
                fn()
        assert not deferred

    xres_t = [xres.reg.sub(t) for t in range(NT)]

    def phase_E(l, xsrc, xsrc_regs):
        A.top = A_BASE
        wo = A.carve([8, D], BF16, "wo")
        P.dma("sp", wo.ap, w_out_b[l].ap.rearrange("(c p) n -> p c n", p=128), reads=[w_out_b[l].reg], writes=[wo.reg])
        mts = [A.carve([8, TT], BF16, "mt%d" % i) for i in range(2)]
        xts = [A.carve([4, D], F32, "xte%d" % i) for i in range(2)]
        k = 0
        for t in range(NT):
            mt, xt = mts[t % 2], xts[t % 2]
            P.dma("sp", mt.ap, mixedT.ap[:, t * TT:(t + 1) * TT].rearrange("(c p) n -> p c n", p=128),
                  reads=[mixedT.reg], writes=[mt.reg])
            P.dma("sp", xt.ap, xsrc[t * TT:(t + 1) * TT, :].rearrange("(s p) d -> p s d", p=128),
                  reads=[xsrc_regs[t]], writes=[xt.reg])
            for s in range(4):
                for half in range(2):
                    bk = banks[k % 8]
                    k += 1
                    for c in range(8):
                        P.op("pe", lambda e, bk=bk, mt=mt, c=c, s=s, half=half: e.matmul(
                            bk.ap, lhsT=mt.ap[:, c, s * 128:(s + 1) * 128], rhs=wo.ap[:, c, half * 512:(half + 1) * 512],
                            start=(c == 0), stop=(c == 7)), reads=[mt.reg, wo.reg], writes=[bk.reg])
                    P.op("dve", lambda e, bk=bk, xt=xt, s=s, half=half: e.tensor_tensor(
                        out=xt.ap[:, s, half * 512:(half + 1) * 512], in0=bk.ap, in1=xt.ap[:, s, half * 512:(half + 1) * 512],
                        op=ALU.add), reads=[bk.reg, xt.reg], writes=[xt.reg])
            P.dma("sp", xres.ap[t * TT:(t + 1) * TT, :].rearrange("(s p) d -> p s d", p=128), xt.ap,
                  reads=[xt.reg], writes=[xres_t[t]])

    def phase_F(l, moe):
        A.top = A_BASE
        E = NE if moe else 1
        w1s = mw1_b if moe else fw1_b
        w3s = mw3_b if moe else fw3_b
        w2s = mw2_b if moe else fw2_b
        g_rep = A.carve([1, D], F32, "gf_rep")
        P.dma("sp", g_rep.ap, norm_ffn_g[l:l + 1, :].partition_broadcast(128), writes=[g_rep.reg])
        if moe:
            gfin = A.carve([1, D], F32, "gfin")
            P.dma("sp", gfin.ap, final_norm_g[0:1, :].partition_broadcast(128), writes=[gfin.reg])
            Rsb = A.carve([8, NE], F32, "Rsb")
            import os as _os3
            if not _os3.environ.get("K_NORSB"):
                P.dma("sp", Rsb.ap, moe_router[0].rearrange("(c p) e -> p c e", p=128), writes=[Rsb.reg])
            hT32 = A.carve([8, TT], F32, "hT32")
            idxs = A.carve([16], U32, "idxs")
            P.dma("sp", idxs.ap, tok_idx, writes=[idxs.reg])
            Gt = A.carve([4, NE], F32, "Gt")
            lgs, eq1, eq2, lg2 = [A.carve([NE], F32, n) for n in ("lgs", "eq1", "eq2", "lg2")]
            m1, m2, dd, g1, g2 = [A.carve([1], F32, n) for n in ("m1", "m2", "dd", "g1", "g2")]
        xt = A.carve([4, D], F32, "xtf")
        h32 = A.carve([4, D], F32, "h32")
        hT = A.carve([8, TT], BF16, "hT")
        actT = A.carve([NF, TT], BF16, "actT")
        W2s = [A.carve([NF, D], BF16, "W2s0")]
        GT = 4
        NW = 3
        w13 = [A.carve([8, 2, GT * 128], BF16, "w13_%d" % i) for i in range(NW)]
        w13r = [(w.reg.sub("w1"), w.reg.sub("w3")) for w in w13]
        sa = [A.carve([TT], BF16, "sa%d" % i) for i in range(2)]
        junk = A.carve([D], BF16, "junkf")
        ss, sd, rstd = [A.carve([4], F32, n) for n in ("ssf", "sdf", "rstdf")]

        import os as _os
        _nt = int(_os.environ.get("K_NT_F", NT // 2)) if moe else NT
        _ne = int(_os.environ.get("K_NE", E)) if moe else E
        iters = [(t, e_) for t in range(_nt) for e_ in range(_ne)]
        E = _ne
        NG = (NF + GT - 1) // GT
        glist = [(i_, g_) for i_ in range(len(iters)) for g_ in range(NG)]

        def load_w2(i):
            t, e_ = iters[i]
            w = W2s[0]
            P.dma("sp", w.ap, w2s[e_].ap.rearrange("(j p) n -> p j n", p=128), reads=[w2s[e_].reg], writes=[w.reg])

        gcount = [0]

        def load_w13(gi):
            i, grp = glist[gi]
            t, e_ = iters[i]
            w = w13[gi % NW]
            c0 = grp * GT * 128
            c1 = min(FF, c0 + GT * 128)
            P.dma("sp", w.ap[:, :, 0, 0:c1 - c0], w1s[e_].ap[:, c0:c1].rearrange("(c p) f -> p c f", p=128),
                  reads=[w1s[e_].reg], writes=[w13r[gi % NW][0]])
            P.dma("sp", w.ap[:, :, 1, 0:c1 - c0], w3s[e_].ap[:, c0:c1].rearrange("(c p) f -> p c f", p=128),
                  reads=[w3s[e_].reg], writes=[w13r[gi % NW][1]])

        load_w13(0)
        load_w13(1)
        kab = 0
        kw2 = 0
        w2banks = (0, 1, 6, 7)
        for i, (t, e_) in enumerate(iters):
            if e_ == 0:
                if moe:
                    for s in range(4):
                        P._add("pool", lambda e, s=s, t=t: e.indirect_dma_start(
                            out=xt.ap[:, s, :], out_offset=None, in_=xres.ap[:, :],
                            in_offset=bass.IndirectOffsetOnAxis(ap=idxs.ap[:, t * 4 + s:t * 4 + s + 1], axis=0)),
                            [xres.reg, idxs.reg], [xt.reg], True)
                else:
                    P.dma("sp", xt.ap, xres.ap[t * TT:(t + 1) * TT, :].rearrange("(s p) d -> p s d", p=128),
                          reads=[xres_t[t]], writes=[xt.reg])
                for s in range(4):
                    rmsnorm_rows(xt, s, ss, sd, rstd, junk, D)
                P.op("act", lambda e: e.activation(out=sd.ap, in_=ss.ap, func=AF.Sqrt, scale=1.0 / D, bias=EPS),
                     reads=[ss.reg], writes=[sd.reg])
                P.op("dve", lambda e: e.reciprocal(out=rstd.ap, in_=sd.ap), reads=[sd.reg], writes=[rstd.reg])
                for s in range(4):
                    P.op("dve", lambda e, s=s: e.scalar_tensor_tensor(
                        out=h32.ap[:, s, :], in0=xt.ap[:, s, :], scalar=rstd.ap[:, s:s + 1], in1=g_rep.ap[:, 0, :],
                        op0=ALU.mult, op1=ALU.mult), reads=[xt.reg, rstd.reg, g_rep.reg], writes=[h32.reg])
                for c in range(8):
                    bk = banks[c % 2]
                    for s in range(4):
                        P.op("pe", lambda e, bk=bk, s=s, c=c: e.transpose(
                            out=bk.ap[:, s * 128:(s + 1) * 128], in_=h32.ap[:, s, c * 128:(c + 1) * 128],
                            identity=ident_f.ap), reads=[h32.reg, ident_f.reg], writes=[bk.reg])
                    P.op("act", lambda e, bk=bk, c=c: e.activation(out=hT.ap[:, c, :], in_=bk.ap, func=AF.Copy),
                         reads=[bk.reg], writes=[hT.reg])
                    if moe:
                        P.op("dve", lambda e, bk=bk, c=c: e.tensor_copy(out=hT32.ap[:, c, :], in_=bk.ap),
                             reads=[bk.reg], writes=[hT32.reg, bk.reg])
                _skip = _os.environ.get("K_SKIP", "")
                if moe and _skip:
                    P.op("dve", lambda e: e.memset(Gt.ap, 0.125), writes=[Gt.reg])
                if moe and not _skip:
                    lb = banks[2]
                    for s in range(4):
                        for c in range(8):
                            P.op("pe", lambda e, s=s, c=c: e.matmul(
                                lb.ap[:, s * 8:(s + 1) * 8], lhsT=hT32.ap[:, c, s * 128:(s + 1) * 128], rhs=Rsb.ap[:, c, :],
                                start=(c == 0), stop=(c == 7)), reads=[hT32.reg, Rsb.reg], writes=[lb.reg])
                        P.op("dve", lambda e, s=s: e.tensor_copy(out=lgs.ap, in_=lb.ap[:, s * 8:(s + 1) * 8]),
                             reads=[lb.reg], writes=[lgs.reg])
                        P.op("dve", lambda e: e.reduce_max(out=m1.ap, in_=lgs.ap, axis=AX.X), reads=[lgs.reg], writes=[m1.reg])
                        P.op("dve", lambda e: e.tensor_scalar(out=eq1.ap, in0=lgs.ap, scalar1=m1.ap[:, 0:1], scalar2=None,
                                                              op0=ALU.is_equal), reads=[lgs.reg, m1.reg], writes=[eq1.reg])
                        P.op("dve", lambda e: e.scalar_tensor_tensor(out=lg2.ap, in0=eq1.ap, scalar=-1e30, in1=lgs.ap,
                                                                     op0=ALU.mult, op1=ALU.add),
                             reads=[eq1.reg, lgs.reg], writes=[lg2.reg])
                        P.op("dve", lambda e: e.reduce_max(out=m2.ap, in_=lg2.ap, axis=AX.X), reads=[lg2.reg], writes=[m2.reg])
                        P.op("dve", lambda e: e.tensor_scalar(out=eq2.ap, in0=lg2.ap, scalar1=m2.ap[:, 0:1], scalar2=None,
                                                              op0=ALU.is_equal), reads=[lg2.reg, m2.reg], writes=[eq2.reg])
                        P.op("dve", lambda e: e.tensor_tensor(out=dd.ap, in0=m2.ap, in1=m1.ap, op=ALU.subtract),
                             reads=[m1.reg, m2.reg], writes=[dd.reg])
                        P.op("act", lambda e: e.activation(out=g2.ap, in_=dd.ap, func=AF.Sigmoid), reads=[dd.reg], writes=[g2.reg])
                        P.op("act", lambda e: e.activation(out=g1.ap, in_=dd.ap, func=AF.Sigmoid, scale=-1.0),
                             reads=[dd.reg], writes=[g1.reg])
                        P.op("dve", lambda e, s=s: e.tensor_scalar(out=Gt.ap[:, s, :], in0=eq1.ap, scalar1=g1.ap[:, 0:1], scalar2=None,
                                                                   op0=ALU.mult), reads=[eq1.reg, g1.reg], writes=[Gt.reg])
                        P.op("dve", lambda e, s=s: e.scalar_tensor_tensor(out=Gt.ap[:, s, :], in0=eq2.ap, scalar=g2.ap[:, 0:1],
                                                                          in1=Gt.ap[:, s, :], op0=ALU.mult, op1=ALU.add),
                             reads=[eq2.reg, g2.reg, Gt.reg], writes=[Gt.reg])
            _stop = int(_os.environ.get("K_STOP", "9")) if moe else 9
            if _stop <= 1:
                continue
            load_w2(i)
            W2 = W2s[0]
            for grp in range(NG):
                gi = i * NG + grp
                if gi + 2 < len(glist):
                    load_w13(gi + 2)
                w = w13[gi % NW]
                wr1, wr3 = w13r[gi % NW]
                for jj in range(min(GT, NF - grp * GT)):
                    j = grp * GT + jj
                    ba_, bb_ = banks[2 + (kab % 2) * 2], banks[3 + (kab % 2) * 2]
                    sab = sa[kab % 2]
                    kab += 1
                    for c in range(8):
                        P.op("pe", lambda e, ba_=ba_, w=w, c=c, jj=jj: e.matmul(
                            ba_.ap, lhsT=w.ap[:, c, 0, jj * 128:(jj + 1) * 128], rhs=hT.ap[:, c, :],
                            start=(c == 0), stop=(c == 7)), reads=[wr1, hT.reg], writes=[ba_.reg])
                    for c in range(8):
                        P.op("pe", lambda e, bb_=bb_, w=w, c=c, jj=jj: e.matmul(
                            bb_.ap, lhsT=w.ap[:, c, 1, jj * 128:(jj + 1) * 128], rhs=hT.ap[:, c, :],
                            start=(c == 0), stop=(c == 7)), reads=[wr3, hT.reg], writes=[bb_.reg])
                    P.op("act", lambda e, ba_=ba_, sab=sab: e.activation(out=sab.ap, in_=ba_.ap, func=AF.Silu),
                         reads=[ba_.reg], writes=[sab.reg])
                    P.op("dve", lambda e, bb_=bb_, sab=sab, j=j: e.tensor_tensor(
                        out=actT.ap[:, j, :], in0=bb_.ap, in1=sab.ap, op=ALU.mult),
                        reads=[bb_.reg, sab.reg], writes=[actT.reg])
            if _stop <= 2:
                continue
            for s in range(4):
                for half in range(2):
                    bk = banks[w2banks[kw2 % 4]]
                    kw2 += 1
                    for j in range(NF):
                        P.op("pe", lambda e, bk=bk, j=j, s=s, half=half, W2=W2: e.matmul(
                            bk.ap, lhsT=actT.ap[:, j, s * 128:(s + 1) * 128], rhs=W2.ap[:, j, half * 512:(half + 1) * 512],
                            start=(j == 0), stop=(j == NF - 1)), reads=[actT.reg, W2.reg], writes=[bk.reg])
                    xs = xt.ap[:, s, half * 512:(half + 1) * 512]
                    if moe:
                        P.op("dve", lambda e, bk=bk, xs=xs, s=s, e_=e_: e.scalar_tensor_tensor(
                            out=xs, in0=bk.ap, scalar=Gt.ap[:, s, e_:e_ + 1], in1=xs, op0=ALU.mult, op1=ALU.add),
                            reads=[bk.reg, Gt.reg, xt.reg], writes=[xt.reg])
                    else:
                        P.op("dve", lambda e, bk=bk, xs=xs: e.tensor_tensor(out=xs, in0=bk.ap, in1=xs, op=ALU.add),
                             reads=[bk.reg, xt.reg], writes=[xt.reg])
            if _stop <= 3:
                continue
            if e_ == E - 1:
                if not moe:
                    P.dma("sp", xres.ap[t * TT:(t + 1) * TT, :].rearrange("(s p) d -> p s d", p=128), xt.ap,
                          reads=[xt.reg], writes=[xres_t[t]])
                else:
                    for s in range(4):
                        rmsnorm_rows(xt, s, ss, sd, rstd, junk, D)
                    P.op("act", lambda e: e.activation(out=sd.ap, in_=ss.ap, func=AF.Sqrt, scale=1.0 / D, bias=EPS),
                         reads=[ss.reg], writes=[sd.reg])
                    P.op("dve", lambda e: e.reciprocal(out=rstd.ap, in_=sd.ap), reads=[sd.reg], writes=[rstd.reg])
                    for s in range(4):
                        P.op("dve", lambda e, s=s: e.scalar_tensor_tensor(
                            out=h32.ap[:, s, :], in0=xt.ap[:, s, :], scalar=rstd.ap[:, s:s + 1], in1=gfin.ap[:, 0, :],
                            op0=ALU.mult, op1=ALU.mult), reads=[xt.reg, rstd.reg, gfin.reg], writes=[h32.reg])
                    o = P.dma("sp", out_t.ap[t * TT:(t + 1) * TT, :].rearrange("(s p) d -> p s d", p=128), h32.ap,
                              reads=[h32.reg], writes=[out_t.reg])
                    state["out_ops"].append(o)

    ORDER = ["A0", "B0", "C0", "D0", "E0", "F0", "A1", "B1", "C1", "D1", "E1", "F1"]
    last = len(ORDER) - 1 if upto == "all" else ORDER.index(upto)
    x_in_regs = [Reg("x_in")] * NT
    for l in range(2):
        xsrc = x_in if l == 0 else xres.ap
        xregs = x_in_regs if l == 0 else xres_t
        xreg_whole = x_in_regs[0] if l == 0 else xres.reg
        if ORDER.index("A%d" % l) <= last:
            qT, kT, vaug, keep_top = phase_A(l, xsrc, xreg_whole)
            P.barrier()
        if ORDER.index("B%d" % l) <= last:
            phase_B(l, qT, kT, vaug, keep_top, casts=(l == 0 and last > ORDER.index("B0")))
            P.barrier()
        if ORDER.index("D%d" % l) <= last:
            phase_D(l)
            while cast_queue and last > ORDER.index("D0"):
                pop_cast()
            P.barrier()
        if ORDER.index("E%d" % l) <= last:
            phase_E(l, xsrc, xregs)
            P.barrier()
        if ORDER.index("F%d" % l) <= last:
            phase_F(l, moe=(l == 1))
            P.barrier()
    finals = state["out_ops"] if state["out_ops"] else [o for o in P.ops if o.is_dma][-24:]
    import os as _os2
    if _os2.environ.get("K_WAITCAST"):
        finals = list(finals) + list(P.dma_ops["pool"])
    P.emit(final_wait_ops=finals)
    es.close()
    return nc


def _make_biasT(rel_bias):
    ki = np.arange(128)[:, None, None]
    j = np.arange(5)[None, :, None]
    qi = np.arange(128)[None, None, :]
    rel = (4 - j) * 128 + qi - ki
    idx = np.clip(rel, -128, 128) + 128
    dc = 2 * (j - 4) + (ki >= 64).astype(np.int64) - (qi >= 64).astype(np.int64)
    valid = (dc >= -8) & (dc <= 0)
    ext = np.concatenate([rel_bias, np.full(rel_bias.shape[:2] + (1,), NEG, np.float32)], axis=-1)
    idx = np.where(valid, idx, 257)
    return np.ascontiguousarray(ext[:, :, idx].reshape(2, 8, 128, 640)).astype(np.float32)


def kernel(**inputs):
    nc = build()
    x = np.asarray(inputs["x"], dtype=np.float32)
    common = {}
    for k, v in inputs.items():
        if k in ("x", "attn_rel_bias", "final_norm_g"):
            continue
        common[k] = np.ascontiguousarray(np.asarray(v, dtype=np.float32))
    common["biasT"] = _make_biasT(np.asarray(inputs["attn_rel_bias"], dtype=np.float32))
    common["final_norm_g"] = np.ascontiguousarray(np.asarray(inputs["final_norm_g"], dtype=np.float32).reshape(1, D))
    n = 8
    in_maps = []
    for c in range(n):
        half = c // 4
        idx = (half * (L // 2) + np.arange(16)[None, :] * 128 + np.arange(128)[:, None]).astype(np.uint32)
        in_maps.append(dict(common, x=np.ascontiguousarray(x[c % 4]), tok_idx=np.ascontiguousarray(idx)))
    res = run_bass_kernel_spmd(nc, in_maps, core_ids=list(range(n)))
    out = np.empty((4, L, D), np.float32)
    for c in range(n):
        half = c // 4
        out[c % 4, half * (L // 2):(half + 1) * (L // 2)] = np.asarray(res.results[c]["out"], dtype=np.float32)
    return out
```

```python
import contextlib
import numpy as np
import concourse.bass as bass
import concourse.mybir as mybir
from concourse.bass_utils import run_bass_kernel_spmd

F32 = mybir.dt.float32
BF16 = mybir.dt.bfloat16
U32 = mybir.dt.uint32
AF = mybir.ActivationFunctionType
ALU = mybir.AluOpType
AX = mybir.AxisListType

L = 4096
D = 1024
TT = 512
NT = L // TT
NS = L // 128
DIN = 2304
FF = 2816
NF = FF // 128
NE = 8
EPS = 1e-6
NEG = -30000.0

COMPUTE = ("pe", "act", "dve", "pool")
QUEUES = ("sp", "act", "pool")
DMA_RING = {"sp": 32, "act": 8, "pool": 4}


class Reg:
    __slots__ = ("name", "lw", "rs", "children", "parent")

    def __init__(self, name, parent=None):
        self.name = name
        self.lw = None
        self.rs = []
        self.children = []
        self.parent = parent
        if parent is not None:
            parent.children.append(self)

    def related(self):
        yield self
        if self.parent is not None:
            yield self.parent
        for c in self.children:
            yield c

    def sub(self, name):
        return Reg(self.name + "." + str(name), self)


class Op:
    __slots__ = ("id", "eng", "fn", "deps", "is_dma", "signal", "semval", "semidx", "qidx")

    def __init__(self, id, eng, fn, is_dma):
        self.id = id
        self.eng = eng
        self.fn = fn
        self.is_dma = is_dma
        self.deps = set()
        self.signal = False
        self.semval = None
        self.semidx = None
        self.qidx = None


class Prog:
    def __init__(self, nc):
        self.nc = nc
        self.ops = []
        self.by_eng = {e: [] for e in ("pe", "act", "dve", "pool", "sp")}
        self.ndma = {e: 0 for e in QUEUES}
        self.dma_ops = {e: [] for e in QUEUES}
        self.pending = {}

    def _add(self, eng, fn, reads, writes, is_dma):
        op = Op(len(self.ops), eng, fn, is_dma)
        deps = set()
        for r in reads:
            for x in r.related():
                if x.lw is not None:
                    deps.add(x.lw)
        for w in writes:
            for x in w.related():
                if x.lw is not None:
                    deps.add(x.lw)
                deps.update(x.rs)
        if eng in self.pending:
            deps |= self.pending.pop(eng)
        latest = {}
        for d in deps:
            dop = self.ops[d]
            if dop.is_dma:
                op.deps.add(d)
                continue
            if (not is_dma) and dop.eng == eng and eng == "pe":
                continue
            if latest.get(dop.eng, -1) < d:
                latest[dop.eng] = d
        op.deps.update(latest.values())
        for r in reads:
            r.rs.append(op.id)
        for w in writes:
            w.lw = op.id
            w.rs = []
            for c in w.children:
                c.lw = op.id
                c.rs = []
        if is_dma:
            op.qidx = self.ndma[eng]
            self.ndma[eng] += 1
            self.dma_ops[eng].append(op)
        self.ops.append(op)
        self.by_eng[eng].append(op)
        return op

    def op(self, eng, fn, reads=(), writes=()):
        return self._add(eng, fn, list(reads), list(writes), False)

    def dma(self, eng, out, in_, reads=(), writes=(), **kw):
        return self._add(eng, lambda e: e.dma_start(out=out, in_=in_, **kw), list(reads), list(writes), True)

    def barrier(self):
        b = set()
        for e in COMPUTE:
            lst = [o for o in self.by_eng[e] if not o.is_dma]
            if lst:
                b.add(lst[-1].id)
        for q in QUEUES:
            for o in self.dma_ops[q][-DMA_RING[q]:]:
                b.add(o.id)
        for e in self.by_eng:
            self.pending[e] = set(b) | self.pending.get(e, set())

    def emit(self, final_wait_ops=()):
        nc = self.nc
        ops = self.ops
        for op in ops:
            for d in op.deps:
                ops[d].signal = True
        for fo in final_wait_ops:
            fo.signal = True
        es = contextlib.ExitStack()
        sems = {e: es.enter_context(nc.semaphore("s_" + e)) for e in COMPUTE}
        dsems = {}
        for q in QUEUES:
            if self.ndma[q] > 0:
                dsems[q] = [es.enter_context(nc.semaphore("d_%s%d" % (q, i))) for i in range(DMA_RING[q])]
        cnt = {e: 0 for e in COMPUTE}
        for op in ops:
            if op.is_dma:
                op.semidx = op.qidx % DMA_RING[op.eng]
                op.semval = 16 * (op.qidx // DMA_RING[op.eng] + 1)
            elif op.signal:
                cnt[op.eng] += 1
                op.semval = cnt[op.eng]
        engobj = {"pe": "tensor", "act": "scalar", "dve": "vector", "pool": "gpsimd", "sp": "sync"}
        block = es.enter_context(nc.Block())

        def make_body(ename):
            def body(e):
                waited = {}
                for op in self.by_eng[ename]:
                    need = {}
                    for d in op.deps:
                        dop = ops[d]
                        key = ("d", dop.eng, dop.semidx) if dop.is_dma else ("c", dop.eng)
                        if need.get(key, 0) < dop.semval:
                            need[key] = dop.semval
                    if op.is_dma and op.qidx >= DMA_RING[op.eng]:
                        key = ("d", op.eng, op.semidx)
                        v = op.semval - 16
                        if need.get(key, 0) < v:
                            need[key] = v
                    for key, v in need.items():
                        if waited.get(key, 0) >= v:
                            continue
                        waited[key] = v
                        if key[0] == "d":
                            e.wait_ge(dsems[key[1]][key[2]], v)
                        else:
                            e.wait_ge(sems[key[1]], v)
                    ins = op.fn(e)
                    if op.is_dma:
                        ins.then_inc(dsems[op.eng][op.semidx], 16)
                    elif op.signal:
                        ins.then_inc(sems[op.eng], 1)
                if ename == "sp":
                    for fo in final_wait_ops:
                        if fo.is_dma:
                            e.wait_ge(dsems[fo.eng][fo.semidx], fo.semval)
                        else:
                            e.wait_ge(sems[fo.eng], fo.semval)
            return body

        for ename in ("sp", "act", "dve", "pool", "pe"):
            if not self.by_eng[ename] and ename != "sp":
                continue
            getattr(block, engobj[ename])(make_body(ename))
        es.close()


class T:
    __slots__ = ("ap", "reg")

    def __init__(self, ap, reg):
        self.ap = ap
        self.reg = reg


class Arena:
    def __init__(self, t, cap):
        self.t = t
        self.cap = cap
        self.top = 0
        self.n = 0

    def carve(self, free, dt, name=None):
        n = 1
        for f in free:
            n *= f
        sz = 2 if dt == BF16 else 4
        nb = (n * sz + 63) // 64 * 64
        off = self.top
        self.top += nb
        assert self.top <= self.cap, ("SBUF arena overflow", self.top, self.cap, name)
        v = self.t[:, off // 4:(off + nb) // 4]
        if dt != F32:
            v = v.bitcast(dt)
        v = v[:, 0:n]
        if len(free) > 1:
            names = "abcde"[:len(free)]
            pat = "p (" + " ".join(names) + ") -> p " + " ".join(names)
            v = v.rearrange(pat, **{k: f for k, f in zip(names, free)})
        self.n += 1
        return T(v, Reg(name or ("t%d" % self.n)))


def _rows(ap2d, width):
    sh = ap2d.shape
    if len(sh) == 2:
        return ap2d.rearrange("a (b c) -> (a b) c", c=width)
    if len(sh) == 3:
        return ap2d.rearrange("l a (b c) -> (l a b) c", c=width)
    raise ValueError


def build(upto="all", dbg=()):
    nc = bass.Bass("TRN2", target_bir_lowering=False)
    es = contextlib.ExitStack()
    P = Prog(nc)

    def din(name, shape):
        return nc.dram_tensor(name, list(shape), F32, kind="ExternalInput").ap()

    def dscr(name, shape, dt, shared=False):
        kind = "ExternalOutput" if name in dbg else "Internal"
        if shared and kind == "Internal":
            return T(nc.dram_tensor(name, list(shape), dt, kind=kind, addr_space="Shared").ap(), Reg(name))
        return T(nc.dram_tensor(name, list(shape), dt, kind=kind).ap(), Reg(name))

    x_in = din("x", [L, D])
    norm_mix_g = din("norm_mix_g", [2, D])
    w_in = din("w_in", [2, D, DIN])
    biasT = din("biasT", [2, 8, 128, 640])
    rg_conv_w = din("rg_conv_w", [2, 4, 256])
    rg_conv_b = din("rg_conv_b", [2, 256])
    rg_wx = din("rg_wx", [2, 4, 64, 64])
    rg_bx = din("rg_bx", [2, 256])
    rg_wa = din("rg_wa", [2, 4, 64, 64])
    rg_ba = din("rg_ba", [2, 256])
    rg_lambda = din("rg_lambda", [2, 256])
    s5_a_re = din("s5_a_re", [2, 16, 64])
    s5_a_im = din("s5_a_im", [2, 16, 64])
    s5_log_dt = din("s5_log_dt", [2, 16])
    s5_b_re = din("s5_b_re", [2, 16, 64, 16])
    s5_b_im = din("s5_b_im", [2, 16, 64, 16])
    s5_c_re = din("s5_c_re", [2, 16, 16, 64])
    s5_c_im = din("s5_c_im", [2, 16, 16, 64])
    s5_d = din("s5_d", [2, 256])
    s5_w_glu = din("s5_w_glu", [2, 256, 256])
    g_group = din("g_group", [2, D])
    w_out = din("w_out", [2, D, D])
    norm_ffn_g = din("norm_ffn_g", [2, D])
    ffn_w1 = din("ffn_w1", [1, D, FF])
    ffn_w3 = din("ffn_w3", [1, D, FF])
    ffn_w2 = din("ffn_w2", [1, FF, D])
    moe_router = din("moe_router", [1, D, NE])
    moe_w1 = din("moe_w1", [1, NE, D, FF])
    moe_w3 = din("moe_w3", [1, NE, D, FF])
    moe_w2 = din("moe_w2", [1, NE, FF, D])
    final_norm_g = din("final_norm_g", [1, D])
    tok_idx = nc.dram_tensor("tok_idx", [128, 16], U32, kind="ExternalInput").ap()
    out_t = T(nc.dram_tensor("out", [L // 2, D], F32, kind="ExternalOutput").ap(), Reg("out"))

    xres = dscr("xres", [L, D], F32)
    rgsT = dscr("rgsT", [768, L], F32)
    mixedT = dscr("mixedT", [D, L], BF16)
    w_in_b = [dscr("w_in_b%d" % l, [D, DIN], BF16) for l in range(2)]
    w_out_b = [dscr("w_out_b%d" % l, [D, D], BF16) for l in range(2)]
    fw1_b = [dscr("fw1_b", [D, FF], BF16)]
    fw3_b = [dscr("fw3_b", [D, FF], BF16)]
    fw2_b = [dscr("fw2_b", [FF, D], BF16)]
    mw1_b = [dscr("mw1_b%d" % e, [D, FF], BF16) for e in range(NE)]
    mw3_b = [dscr("mw3_b%d" % e, [D, FF], BF16, shared=True) for e in range(NE)]
    mw2_b = [dscr("mw2_b%d" % e, [FF, D], BF16, shared=True) for e in range(NE)]

    ARENA_BYTES = 204 * 1024
    arena_t = es.enter_context(nc.sbuf_tensor("arena", [128, ARENA_BYTES // 4], F32))
    A = Arena(arena_t, ARENA_BYTES)
    banks = []
    for i in range(8):
        pt = es.enter_context(nc.psum_tensor("bank%d" % i, [128, 512], F32))
        banks.append(T(pt[:, :], Reg("bank%d" % i)))

    def bank_bf(i):
        return banks[i].ap.bitcast(BF16)

    ident_f = A.carve([128], F32, "ident_f")
    ones_f = A.carve([128], F32, "ones_f")
    ident_b = A.carve([128], BF16, "ident_b")
    P.op("pool", lambda e: e.memset(ident_f.ap, 1.0), writes=[ident_f.reg])
    P.op("pool", lambda e: e.affine_select(out=ident_f.ap, in_=ident_f.ap, pattern=[[-1, 128]],
                                           compare_op=ALU.is_equal, fill=0.0, base=0, channel_multiplier=1),
         reads=[ident_f.reg], writes=[ident_f.reg])
    P.op("pool", lambda e: e.memset(ones_f.ap, 1.0), writes=[ones_f.reg])
    P.op("dve", lambda e: e.tensor_copy(out=ident_b.ap, in_=ident_f.ap), reads=[ident_f.reg], writes=[ident_b.reg])
    def vec2(src_row, name):
        t = A.carve([2], F32, name)
        P.dma("sp", t.ap, src_row.rearrange("(ct p) -> p ct", p=128), writes=[t.reg], allow_slow_non_contiguous=True)
        return t

    rgp = []
    for l_ in range(2):
        cw_ = A.carve([2, 4], F32, "cw%d" % l_)
        for ct in range(2):
            P.dma("sp", cw_.ap[:, ct, :], rg_conv_w[l_][:, ct * 128:(ct + 1) * 128].rearrange("j p -> p j"), writes=[cw_.reg],
                  allow_slow_non_contiguous=True)
        rgp.append(dict(cw=cw_, cb=vec2(rg_conv_b[l_], "cb%d" % l_), bx=vec2(rg_bx[l_], "bx%d" % l_),
                        ba=vec2(rg_ba[l_], "ba%d" % l_), lam=vec2(rg_lambda[l_], "lam%d" % l_),
                        gg=vec2(g_group[l_, 512:768], "ggrg%d" % l_)))
    A_BASE = A.top

    def cast(dst, src, width, after=()):
        P.dma("pool", _rows(dst.ap, width), _rows(src, width), reads=list(after), writes=[dst.reg])

    cast(w_in_b[0], w_in[0], 1152)

    cast_queue = [(w_out_b[0], w_out[0], 1024), (fw1_b[0], ffn_w1[0], 1408), (fw3_b[0], ffn_w3[0], 1408),
                  (fw2_b[0], ffn_w2[0], 1024), (w_in_b[1], w_in[1], 1152), (w_out_b[1], w_out[1], 1024)]
    for e_ in range(NE):
        cast_queue += [(mw1_b[e_], moe_w1[0, e_], 1408), (mw3_b[e_], moe_w3[0, e_], 1408), (mw2_b[e_], moe_w2[0, e_], 1024)]

    def pop_cast(after=()):
        if cast_queue:
            d_, s_, w_ = cast_queue.pop(0)
            cast(d_, s_, w_, after=after)

    def issue_rest_casts(after=()):
        for _ in range(4):
            pop_cast(after=[rgsT.reg] + list(after))

    def rmsnorm_rows(xt, s, ss, sd, rstd, junk, nfeat):
        P.op("act", lambda e: e.activation(out=junk.ap, in_=xt.ap[:, s, :], func=AF.Square,
                                           accum_out=ss.ap[:, s:s + 1]),
             reads=[xt.reg], writes=[junk.reg, ss.reg])

    state = {"out_ops": []}

    def phase_A(l, xsrc, xsrc_reg):
        A.top = A_BASE
        qT = A.carve([4, L], BF16, "qT")
        kT = A.carve([4, L], BF16, "kT")
        vaug = A.carve([NS, 8, 65], BF16, "vaug")
        P.op("dve", lambda e: e.memset(vaug.ap, 1.0), writes=[vaug.reg])
        keep_top = A.top
        wsb = A.carve([8, DIN], BF16, "wsb")
        P.dma("sp", wsb.ap, w_in_b[l].ap.rearrange("(c p) n -> p c n", p=128), reads=[w_in_b[l].reg], writes=[wsb.reg])
        g_rep = A.carve([1, D], F32, "g_rep")
        P.dma("sp", g_rep.ap, norm_mix_g[l:l + 1, :].partition_broadcast(128), writes=[g_rep.reg])
        xts = [A.carve([4, D], F32, "xt%d" % i) for i in range(1)]
        ub = A.carve([4, D], F32, "ub")
        uTs = [A.carve([8, TT], BF16, "uT%d" % i) for i in range(2)]
        junk = A.carve([D], BF16, "junk")
        ss = A.carve([4], F32, "ss")
        sd = A.carve([4], F32, "sd")
        rstd = A.carve([4], F32, "rstd")
        rgst = [A.carve([6, TT], F32, "rgst%d" % i) for i in range(1)]
        nb = [2]

        def next_bank():
            b = banks[nb[0]]
            nb[0] = 2 + (nb[0] - 2 + 1) % 6
            return b

        def prologue(t):
            uT = uTs[t % 2]
            xt = xts[0]
            P.dma("sp", xt.ap, xsrc[t * TT:(t + 1) * TT, :].rearrange("(s p) d -> p s d", p=128),
                  reads=[xsrc_reg], writes=[xt.reg])
            for s in range(4):
                rmsnorm_rows(xt, s, ss, sd, rstd, junk, D)
            P.op("act", lambda e: e.activation(out=sd.ap, in_=ss.ap, func=AF.Sqrt, scale=1.0 / D, bias=EPS),
                 reads=[ss.reg], writes=[sd.reg])
            P.op("dve", lambda e: e.reciprocal(out=rstd.ap, in_=sd.ap), reads=[sd.reg], writes=[rstd.reg])
            for s in range(4):
                P.op("dve", lambda e, s=s, xt=xt: e.scalar_tensor_tensor(
                    out=ub.ap[:, s, :], in0=xt.ap[:, s, :], scalar=rstd.ap[:, s:s + 1], in1=g_rep.ap[:, 0, :],
                    op0=ALU.mult, op1=ALU.mult), reads=[xt.reg, rstd.reg, g_rep.reg], writes=[ub.reg])
            for c in range(8):
                bk = banks[c % 2]
                for s in range(4):
                    P.op("pe", lambda e, bk=bk, s=s, c=c: e.transpose(
                        out=bk.ap[:, s * 128:(s + 1) * 128], in_=ub.ap[:, s, c * 128:(c + 1) * 128],
                        identity=ident_f.ap), reads=[ub.reg, ident_f.reg], writes=[bk.reg])
                P.op("act", lambda e, bk=bk, c=c: e.activation(out=uT.ap[:, c, :], in_=bk.ap, func=AF.Copy),
                     reads=[bk.reg], writes=[uT.reg])
        prologue(0)
        for t in range(NT):
            uT = uTs[t % 2]
            for which, dst, scale in ((0, qT, 0.125), (1, kT, 1.0)):
                for j in range(4):
                    bk = next_bank()
                    col = which * 512 + j * 128
                    for c in range(8):
                        P.op("pe", lambda e, bk=bk, c=c, col=col, uT=uT: e.matmul(
                            bk.ap, lhsT=wsb.ap[:, c, col:col + 128], rhs=uT.ap[:, c, :],
                            start=(c == 0), stop=(c == 7)), reads=[wsb.reg, uT.reg], writes=[bk.reg])
                    P.op("act", lambda e, bk=bk, dst=dst, j=j, t=t, scale=scale: e.activation(
                        out=dst.ap[:, j, t * TT:(t + 1) * TT], in_=bk.ap, func=AF.Copy, scale=scale),
                        reads=[bk.reg], writes=[dst.reg])
            for s in range(4):
                bk = next_bank()
                for c in range(8):
                    P.op("pe", lambda e, bk=bk, c=c, s=s, uT=uT: e.matmul(
                        bk.ap, lhsT=uT.ap[:, c, s * 128:(s + 1) * 128], rhs=wsb.ap[:, c, 1024:1536],
                        start=(c == 0), stop=(c == 7)), reads=[wsb.reg, uT.reg], writes=[bk.reg])
                P.op("dve", lambda e, bk=bk, s=s, t=t: e.tensor_copy(
                    out=vaug.ap[:, t * 4 + s, :, 0:64], in_=bk.ap.rearrange("p (h d) -> p h d", h=8)),
                    reads=[bk.reg], writes=[vaug.reg])
            if t + 1 < NT:
                prologue(t + 1)
            rg = rgst[0]
            for j in range(6):
                bk = next_bank()
                col = 1536 + j * 128
                for c in range(8):
                    P.op("pe", lambda e, bk=bk, c=c, col=col, uT=uT: e.matmul(
                        bk.ap, lhsT=wsb.ap[:, c, col:col + 128], rhs=uT.ap[:, c, :],
                        start=(c == 0), stop=(c == 7)), reads=[wsb.reg, uT.reg], writes=[bk.reg])
                P.op("dve", lambda e, bk=bk, rg=rg, j=j: e.tensor_copy(out=rg.ap[:, j, :], in_=bk.ap),
                     reads=[bk.reg], writes=[rg.reg])
            P.dma("sp", rgsT.ap[:, t * TT:(t + 1) * TT].rearrange("(j p) n -> p j n", p=128), rg.ap,
                  reads=[rg.reg], writes=[rgsT.reg])
        return qT, kT, vaug, keep_top

    def phase_B(l, qT, kT, vaug, keep_top, fuse_c=True, casts=False):
        A.top = keep_top
        bias_sb = A.carve([8, 640], F32, "bias_sb")
        P.dma("sp", bias_sb.ap, biasT[l].rearrange("h k q -> k h q"), writes=[bias_sb.reg])
        gg_rep = A.carve([1, 512], F32, "gg_rep")
        P.dma("sp", gg_rep.ap, g_group[l:l + 1, 0:512].partition_broadcast(128), writes=[gg_rep.reg])
        if casts:
            issue_rest_casts(after=[bias_sb.reg, gg_rep.reg])
        stmp = [A.carve([640], F32, "stmp%d" % i) for i in range(2)]
        pT = [A.carve([640], BF16, "pT%d" % i) for i in range(2)]
        yatt = A.carve([8, 64], F32, "yatt")
        rc = A.carve([8], F32, "rc")
        junk = A.carve([512], BF16, "junkb")
        ss = A.carve([1], F32, "ssb")
        sd = A.carve([1], F32, "sdb")
        rstd = A.carve([1], F32, "rstdb")
        mxa = A.carve([512], F32, "mxa")
        mst = [A.carve([4, TT], BF16, "mst%d" % i) for i in range(2)]
        NIT = NS * 8
        stmp3 = stmp + [A.carve([640], F32, "stmp2")]
        pT3 = pT + [A.carve([640], BF16, "pT2")]

        def kts_of(qt):
            return [kt for kt in range(qt - 4, qt + 1) if kt >= 0]

        def emit_qk(idx):
            qt, h = divmod(idx, 8)
            kts = kts_of(qt)
            pb = (h % 2) * 64
            jp = h // 2
            sA, sB = banks[(idx % 2) * 2], banks[(idx % 2) * 2 + 1]
            for i, kt in enumerate(kts):
                dstb = sA if i < 4 else sB
                ci = i if i < 4 else 0
                P.op("pe", lambda e, dstb=dstb, ci=ci, kt=kt, pb=pb, jp=jp, qt=qt: e.matmul(
                    dstb.ap[:, ci * 128:(ci + 1) * 128],
                    lhsT=kT.ap[pb:pb + 64, jp, kt * 128:(kt + 1) * 128],
                    rhs=qT.ap[pb:pb + 64, jp, qt * 128:(qt + 1) * 128], start=True, stop=True),
                    reads=[kT.reg, qT.reg], writes=[dstb.reg])

        def emit_rest(idx):
            qt, h = divmod(idx, 8)
            kts = kts_of(qt)
            n = len(kts)
            sA, sB = banks[(idx % 2) * 2], banks[(idx % 2) * 2 + 1]
            st, pt_ = stmp3[idx % 3], pT3[idx % 3]
            j0 = kts[0] - qt + 4
            n4 = min(n, 4)
            P.op("dve", lambda e, st=st, sA=sA, h=h, j0=j0, n4=n4: e.tensor_tensor(
                out=st.ap[:, 0:n4 * 128], in0=sA.ap[:, 0:n4 * 128],
                in1=bias_sb.ap[:, h, j0 * 128:(j0 + n4) * 128], op=ALU.add),
                reads=[sA.reg, bias_sb.reg], writes=[st.reg])
            if n == 5:
                P.op("dve", lambda e, st=st, sB=sB, h=h: e.tensor_tensor(
                    out=st.ap[:, 512:640], in0=sB.ap[:, 0:128], in1=bias_sb.ap[:, h, 512:640], op=ALU.add),
                    reads=[sB.reg, bias_sb.reg], writes=[st.reg])
            P.op("act", lambda e, st=st, pt_=pt_, n=n: e.activation(
                out=pt_.ap[:, 0:n * 128], in_=st.ap[:, 0:n * 128], func=AF.Exp),
                reads=[st.reg], writes=[pt_.reg])
            ob = banks[4 + h // 4]
            hh = h % 4
            for i, kt in enumerate(kts):
                P.op("pe", lambda e, ob=ob, hh=hh, i=i, kt=kt, h=h, pt_=pt_, n=n: e.matmul(
                    ob.ap[:, hh * 65:(hh + 1) * 65], lhsT=pt_.ap[:, i * 128:(i + 1) * 128],
                    rhs=vaug.ap[:, kt, h, :], start=(i == 0), stop=(i == n - 1)),
                    reads=[pt_.reg, vaug.reg], writes=[ob.reg])

        cgen = gen_C(l) if fuse_c else iter(())
        emit_qk(0)
        for qt in range(NS):
            for h in range(8):
                idx = qt * 8 + h
                if idx + 1 < NIT:
                    emit_qk(idx + 1)
                emit_rest(idx)
                next(cgen, None)
            for half in range(2):
                ob = banks[4 + half]
                ov = ob.ap[:, 0:260].rearrange("p (h d) -> p h d", h=4)
                P.op("dve", lambda e, ov=ov, half=half: e.reciprocal(
                    out=rc.ap[:, half * 4:(half + 1) * 4], in_=ov[:, :, 64]), reads=[ob.reg], writes=[rc.reg])
                P.op("dve", lambda e, ov=ov, half=half: e.tensor_tensor(
                    out=yatt.ap[:, half * 4:(half + 1) * 4, :], in0=ov[:, :, 0:64],
                    in1=rc.ap[:, half * 4:(half + 1) * 4].unsqueeze(2).to_broadcast([128, 4, 64]), op=ALU.mult),
                    reads=[ob.reg, rc.reg], writes=[yatt.reg])
            yflat = yatt.ap.rearrange("p h d -> p (h d)")
            P.op("act", lambda e, yflat=yflat: e.activation(out=junk.ap, in_=yflat, func=AF.Square, accum_out=ss.ap),
                 reads=[yatt.reg], writes=[junk.reg, ss.reg])
            P.op("act", lambda e: e.activation(out=sd.ap, in_=ss.ap, func=AF.Ln, scale=1.0 / 512, bias=EPS),
                 reads=[ss.reg], writes=[sd.reg])
            P.op("act", lambda e: e.activation(out=rstd.ap, in_=sd.ap, func=AF.Exp, scale=-0.5), reads=[sd.reg], writes=[rstd.reg])
            P.op("dve", lambda e, yflat=yflat: e.scalar_tensor_tensor(
                out=mxa.ap, in0=yflat, scalar=rstd.ap[:, 0:1], in1=gg_rep.ap[:, 0, :], op0=ALU.mult, op1=ALU.mult),
                reads=[yatt.reg, rstd.reg, gg_rep.reg], writes=[mxa.reg])
            tb = banks[6]
            for c in range(4):
                P.op("pe", lambda e, c=c: e.transpose(out=tb.ap[:, c * 128:(c + 1) * 128],
                                                      in_=mxa.ap[:, c * 128:(c + 1) * 128], identity=ident_f.ap),
                     reads=[mxa.reg, ident_f.reg], writes=[tb.reg])
            ms = mst[(qt // 4) % 2]
            q4 = qt % 4
            P.op("act", lambda e, ms=ms, q4=q4: e.activation(
                out=ms.ap[:, :, q4 * 128:(q4 + 1) * 128], in_=tb.ap.rearrange("p (c t) -> p c t", c=4), func=AF.Copy),
                reads=[tb.reg], writes=[ms.reg])
            if q4 == 3:
                t = qt // 4
                P.dma("sp", mixedT.ap[0:512, t * TT:(t + 1) * TT].rearrange("(c p) n -> p c n", p=128), ms.ap,
                      reads=[ms.reg], writes=[mixedT.reg])
        for _ in cgen:
            pass

    def gen_C(l):
        CH = 1024
        NCH = L // CH
        cw, cb, bx, ba, lam, gg = (rgp[l][k] for k in ("cw", "cb", "bx", "ba", "lam", "gg"))
        sp_ = A.carve([2], F32, "sp_")
        c8 = A.carve([2], F32, "c8")
        c16 = A.carve([2], F32, "c16")
        P.op("act", lambda e: e.activation(out=sp_.ap, in_=lam.ap, func=AF.Exp, scale=-1.0), reads=[lam.reg], writes=[sp_.reg])
        P.op("act", lambda e: e.activation(out=sp_.ap, in_=sp_.ap, func=AF.Ln, bias=1.0), reads=[sp_.reg], writes=[sp_.reg])
        P.op("dve", lambda e: e.tensor_scalar(out=c8.ap, in0=sp_.ap, scalar1=-8.0, scalar2=None, op0=ALU.mult),
             reads=[sp_.reg], writes=[c8.reg])
        P.op("dve", lambda e: e.tensor_scalar(out=c16.ap, in0=sp_.ap, scalar1=-16.0, scalar2=None, op0=ALU.mult),
             reads=[sp_.reg], writes=[c16.reg])
        wf = A.carve([2, 2, 128], F32, "wf_bd")
        wb = A.carve([2, 2, 128], BF16, "wb_bd")
        P.op("dve", lambda e: e.memset(wf.ap, 0.0), writes=[wf.reg])
        for gi, src in enumerate((rg_wx, rg_wa)):
            for ct in range(2):
                for blk in range(2):
                    P.dma("sp", wf.ap[blk * 64:(blk + 1) * 64, gi, ct, blk * 64:(blk + 1) * 64], src[l, 2 * ct + blk],
                          writes=[wf.reg])
        P.op("dve", lambda e: e.tensor_copy(out=wb.ap, in_=wf.ap), reads=[wf.reg], writes=[wb.reg])
        yield
        xr = A.carve([CH + 3], F32, "xr")
        gate = A.carve([CH], F32, "gate")
        xc = A.carve([CH], F32, "xc")
        xcb = A.carve([CH], BF16, "xcb")
        gx = A.carve([CH], F32, "gx")
        ga = A.carve([CH], F32, "ga")
        aa = A.carve([CH], F32, "aa")
        hh = A.carve([CH], F32, "hh")
        ys = [A.carve([CH], F32, "yrg%d" % i) for i in range(2)]
        sq = A.carve([TT], F32, "sqrg")
        sdc = A.carve([TT], F32, "sdc")
        rstdc = A.carve([TT], F32, "rstdc")
        mo = A.carve([2, TT], BF16, "morg")
        hcar = A.carve([2], F32, "hcar")
        P.op("dve", lambda e: e.memset(hcar.ap, 0.0), writes=[hcar.reg])
        bk = banks[7]
        for ch in range(NCH):
            t0 = ch * CH
            for ct in range(2):
                r0 = ct * 128
                if ch == 0:
                    P.op("dve", lambda e: e.memset(xr.ap[:, 0:3], 0.0), writes=[xr.reg])
                    P.dma("sp", xr.ap[:, 3:CH + 3], rgsT.ap[r0:r0 + 128, 0:CH], reads=[rgsT.reg], writes=[xr.reg])
                else:
                    P.dma("sp", xr.ap, rgsT.ap[r0:r0 + 128, t0 - 3:t0 + CH], reads=[rgsT.reg], writes=[xr.reg])
                P.dma("sp", gate.ap, rgsT.ap[256 + r0:256 + r0 + 128, t0:t0 + CH], reads=[rgsT.reg], writes=[gate.reg])
                P.op("dve", lambda e, ct=ct: e.tensor_scalar(out=xc.ap, in0=xr.ap[:, 3:CH + 3], scalar1=cw.ap[:, ct, 3:4],
                                                             scalar2=cb.ap[:, ct:ct + 1], op0=ALU.mult, op1=ALU.add),
                     reads=[xr.reg, cw.reg, cb.reg], writes=[xc.reg])
                yield
                for k in range(1, 4):
                    P.op("dve", lambda e, ct=ct, k=k: e.scalar_tensor_tensor(
                        out=xc.ap, in0=xr.ap[:, 3 - k:CH + 3 - k], scalar=cw.ap[:, ct, 3 - k:4 - k], in1=xc.ap,
                        op0=ALU.mult, op1=ALU.add), reads=[xr.reg, cw.reg, xc.reg], writes=[xc.reg])
                    yield
                P.op("dve", lambda e: e.tensor_copy(out=xcb.ap, in_=xc.ap), reads=[xc.reg], writes=[xcb.reg])
                yield
                for gi, (bvec, dst) in enumerate(((bx, gx), (ba, ga))):
                    for piece in range(CH // TT):
                        sl = slice(piece * TT, (piece + 1) * TT)
                        P.op("pe", lambda e, gi=gi, ct=ct, sl=sl: e.matmul(
                            bk.ap, lhsT=wb.ap[:, gi, ct, :], rhs=xcb.ap[:, sl], start=True, stop=True),
                            reads=[wb.reg, xcb.reg], writes=[bk.reg])
                        P.op("act", lambda e, bvec=bvec, dst=dst, ct=ct, sl=sl: e.activation(
                            out=dst.ap[:, sl], in_=bk.ap, func=AF.Sigmoid, bias=bvec.ap[:, ct:ct + 1]),
                            reads=[bk.reg, bvec.reg], writes=[dst.reg])
                yield
                P.op("act", lambda e, ct=ct: e.activation(out=aa.ap, in_=ga.ap, func=AF.Exp, scale=c8.ap[:, ct:ct + 1]),
                     reads=[ga.reg, c8.reg], writes=[aa.reg])
                P.op("act", lambda e, ct=ct: e.activation(out=ga.ap, in_=ga.ap, func=AF.Exp, scale=c16.ap[:, ct:ct + 1]),
                     reads=[ga.reg, c16.reg], writes=[ga.reg])
                yield
                P.op("act", lambda e: e.activation(out=ga.ap, in_=ga.ap, func=AF.Ln, scale=-1.0, bias=1.0),
                     reads=[ga.reg], writes=[ga.reg])
                P.op("act", lambda e: e.activation(out=ga.ap, in_=ga.ap, func=AF.Exp, scale=0.5),
                     reads=[ga.reg], writes=[ga.reg])
                yield
                P.op("dve", lambda e: e.tensor_tensor(out=gx.ap, in0=gx.ap, in1=ga.ap, op=ALU.mult),
                     reads=[gx.reg, ga.reg], writes=[gx.reg])
                yield
                P.op("dve", lambda e: e.tensor_tensor(out=gx.ap, in0=gx.ap, in1=xc.ap, op=ALU.mult),
                     reads=[gx.reg, xc.reg], writes=[gx.reg])
                yield
                P.op("dve", lambda e, ct=ct: e.tensor_tensor_scan(out=hh.ap, data0=aa.ap, data1=gx.ap, initial=hcar.ap[:, ct:ct + 1],
                                                                  op0=ALU.mult, op1=ALU.add),
                     reads=[aa.reg, gx.reg, hcar.reg], writes=[hh.reg])
                P.op("act", lambda e: e.activation(out=gate.ap, in_=gate.ap, func=AF.Gelu_apprx_tanh),
                     reads=[gate.reg], writes=[gate.reg])
                yield
                P.op("dve", lambda e, ct=ct: e.tensor_copy(out=hcar.ap[:, ct:ct + 1], in_=hh.ap[:, CH - 1:CH]),
                     reads=[hh.reg], writes=[hcar.reg])
                y = ys[ct]
                P.op("dve", lambda e, y=y: e.tensor_tensor(out=y.ap, in0=hh.ap, in1=gate.ap, op=ALU.mult),
                     reads=[hh.reg, gate.reg], writes=[y.reg])
                yield
            for piece in range(CH // TT):
                sl = slice(piece * TT, (piece + 1) * TT)
                gsl = slice(t0 + piece * TT, t0 + (piece + 1) * TT)
                for ct in range(2):
                    P.op("dve", lambda e, ct=ct, sl=sl: e.tensor_tensor(out=sq.ap, in0=ys[ct].ap[:, sl], in1=ys[ct].ap[:, sl], op=ALU.mult),
                         reads=[ys[ct].reg], writes=[sq.reg])
                    P.op("pe", lambda e, ct=ct: e.matmul(bk.ap, lhsT=ones_f.ap, rhs=sq.ap, start=(ct == 0), stop=(ct == 1)),
                         reads=[ones_f.reg, sq.reg], writes=[bk.reg])
                    yield
                P.op("act", lambda e: e.activation(out=sdc.ap, in_=bk.ap, func=AF.Ln, scale=1.0 / 256, bias=EPS),
                     reads=[bk.reg], writes=[sdc.reg])
                P.op("act", lambda e: e.activation(out=rstdc.ap, in_=sdc.ap, func=AF.Exp, scale=-0.5), reads=[sdc.reg], writes=[rstdc.reg])
                yield
                for ct in range(2):
                    P.op("dve", lambda e, ct=ct, sl=sl: e.scalar_tensor_tensor(
                        out=mo.ap[:, ct, :], in0=ys[ct].ap[:, sl], scalar=gg.ap[:, ct:ct + 1], in1=rstdc.ap,
                        op0=ALU.mult, op1=ALU.mult), reads=[ys[ct].reg, gg.reg, rstdc.reg], writes=[mo.reg])
                P.dma("sp", mixedT.ap[512:768, gsl].rearrange("(c p) n -> p c n", p=128), mo.ap,
                      reads=[mo.reg], writes=[mixedT.reg])
                yield

    def phase_D(l):
        A.top = A_BASE
        G = 16
        TWO_PI = 6.283185307179586
        C1 = 6.28125
        C2 = TWO_PI - C1

        def small(name, free=(G,)):
            return A.carve(list(free), F32, name)

        are, aim, ldt = small("are"), small("aim"), small("ldt")
        for half in range(2):
            P.dma("sp", are.ap[half * 64:(half + 1) * 64, :], s5_a_re[l].rearrange("g p -> p g"), writes=[are.reg],
                  allow_slow_non_contiguous=True)
            P.dma("sp", aim.ap[half * 64:(half + 1) * 64, :], s5_a_im[l].rearrange("g p -> p g"), writes=[aim.reg],
                  allow_slow_non_contiguous=True)
        P.dma("sp", ldt.ap.rearrange("p (o g) -> p o g", o=1), s5_log_dt[l:l + 1, :].partition_broadcast(128),
              writes=[ldt.reg])
        dvec = vec2(s5_d[l], "dvec")
        gg = vec2(g_group[l, 768:1024], "ggs5")
        dt_ = small("dt_")
        rr = small("rr")
        th = small("th")
        kk = small("kk")
        tmp = small("tmp")
        tmp2 = small("tmp2")
        cs = [small("cs_c"), small("cs_s")]
        cs2 = [small("cs2_c"), small("cs2_s")]

        def dv(fn, reads, writes):
            P.op("dve", fn, reads=[r.reg for r in reads], writes=[w.reg for w in writes])

        P.op("act", lambda e: e.activation(out=dt_.ap, in_=ldt.ap, func=AF.Exp), reads=[ldt.reg], writes=[dt_.reg])
        dv(lambda e: e.tensor_tensor(out=tmp.ap, in0=are.ap, in1=dt_.ap, op=ALU.mult), [are, dt_], [tmp])
        P.op("act", lambda e: e.activation(out=rr.ap, in_=tmp.ap, func=AF.Exp), reads=[tmp.reg], writes=[rr.reg])
        dv(lambda e: e.tensor_tensor(out=th.ap, in0=aim.ap, in1=dt_.ap, op=ALU.mult), [aim, dt_], [th])
        dv(lambda e: e.tensor_scalar(out=kk.ap, in0=th.ap, scalar1=3.141592653589793, scalar2=None, op0=ALU.is_gt), [th], [kk])
        for j in range(1, 5):
            dv(lambda e, j=j: e.tensor_scalar(out=tmp.ap, in0=th.ap, scalar1=(2 * j + 1) * 3.141592653589793, scalar2=None,
                                              op0=ALU.is_gt), [th], [tmp])
            dv(lambda e: e.tensor_tensor(out=kk.ap, in0=kk.ap, in1=tmp.ap, op=ALU.add), [kk, tmp], [kk])
        thr = small("thr")
        dv(lambda e: e.scalar_tensor_tensor(out=thr.ap, in0=kk.ap, scalar=-C1, in1=th.ap, op0=ALU.mult, op1=ALU.add), [kk, th], [thr])
        dv(lambda e: e.scalar_tensor_tensor(out=thr.ap, in0=kk.ap, scalar=-C2, in1=thr.ap, op0=ALU.mult, op1=ALU.add), [kk, thr], [thr])
        P.op("act", lambda e: e.activation(out=cs[1].ap, in_=thr.ap, func=AF.Sin), reads=[thr.reg], writes=[cs[1].reg])
        thc = small("thc")
        dv(lambda e: e.tensor_scalar(out=tmp.ap, in0=thr.ap, scalar1=3.141592653589793 / 2, scalar2=None, op0=ALU.is_gt), [thr], [tmp])
        dv(lambda e: e.scalar_tensor_tensor(out=thc.ap, in0=tmp.ap, scalar=-C1, in1=thr.ap, op0=ALU.mult, op1=ALU.add), [tmp, thr], [thc])
        dv(lambda e: e.scalar_tensor_tensor(out=thc.ap, in0=tmp.ap, scalar=-C2, in1=thc.ap, op0=ALU.mult, op1=ALU.add), [tmp, thc], [thc])
        P.op("act", lambda e: e.activation(out=cs[0].ap, in_=thc.ap, func=AF.Sin, bias=3.141592653589793 / 2),
             reads=[thc.reg], writes=[cs[0].reg])
        nr, ni, den, cre, cim = small("nr"), small("ni"), small("den"), small("cre"), small("cim")
        dv(lambda e: e.tensor_tensor(out=nr.ap, in0=rr.ap, in1=cs[0].ap, op=ALU.mult), [rr, cs[0]], [nr])
        dv(lambda e: e.tensor_scalar(out=nr.ap, in0=nr.ap, scalar1=-1.0, scalar2=None, op0=ALU.add), [nr], [nr])
        dv(lambda e: e.tensor_tensor(out=ni.ap, in0=rr.ap, in1=cs[1].ap, op=ALU.mult), [rr, cs[1]], [ni])
        dv(lambda e: e.tensor_tensor(out=den.ap, in0=are.ap, in1=are.ap, op=ALU.mult), [are], [den])
        dv(lambda e: e.tensor_tensor(out=tmp.ap, in0=aim.ap, in1=aim.ap, op=ALU.mult), [aim], [tmp])
        dv(lambda e: e.tensor_tensor(out=den.ap, in0=den.ap, in1=tmp.ap, op=ALU.add), [den, tmp], [den])
        dv(lambda e: e.reciprocal(out=den.ap, in_=den.ap), [den], [den])
        dv(lambda e: e.tensor_tensor(out=cre.ap, in0=nr.ap, in1=are.ap, op=ALU.mult), [nr, are], [cre])
        dv(lambda e: e.tensor_tensor(out=tmp.ap, in0=ni.ap, in1=aim.ap, op=ALU.mult), [ni, aim], [tmp])
        dv(lambda e: e.tensor_tensor(out=cre.ap, in0=cre.ap, in1=tmp.ap, op=ALU.add), [cre, tmp], [cre])
        dv(lambda e: e.tensor_tensor(out=cre.ap, in0=cre.ap, in1=den.ap, op=ALU.mult), [cre, den], [cre])
        dv(lambda e: e.tensor_tensor(out=cim.ap, in0=ni.ap, in1=are.ap, op=ALU.mult), [ni, are], [cim])
        dv(lambda e: e.tensor_tensor(out=tmp.ap, in0=nr.ap, in1=aim.ap, op=ALU.mult), [nr, aim], [tmp])
        dv(lambda e: e.tensor_tensor(out=cim.ap, in0=cim.ap, in1=tmp.ap, op=ALU.subtract), [cim, tmp], [cim])
        dv(lambda e: e.tensor_tensor(out=cim.ap, in0=cim.ap, in1=den.ap, op=ALU.mult), [cim, den], [cim])
        bre = A.carve([G, 16], F32, "bre")
        bim = A.carve([G, 16], F32, "bim")
        for half in range(2):
            P.dma("sp", bre.ap[half * 64:(half + 1) * 64], s5_b_re[l].rearrange("g p c -> p g c"), writes=[bre.reg])
            P.dma("sp", bim.ap[half * 64:(half + 1) * 64], s5_b_im[l].rearrange("g p c -> p g c"), writes=[bim.reg])
        ta, tb_, tc_, td = [A.carve([G, 16], F32, "t%s" % n) for n in "abcd"]
        X1 = A.carve([G, 16], F32, "X1")
        X2 = A.carve([G, 16], F32, "X2")

        def bc(t, m):
            return t.ap.unsqueeze(2).to_broadcast([128, G, m])

        dv(lambda e: e.tensor_tensor(out=ta.ap, in0=bre.ap, in1=bc(cre, 16), op=ALU.mult), [bre, cre], [ta])
        dv(lambda e: e.tensor_tensor(out=tb_.ap, in0=bim.ap, in1=bc(cim, 16), op=ALU.mult), [bim, cim], [tb_])
        dv(lambda e: e.tensor_tensor(out=tc_.ap, in0=bim.ap, in1=bc(cre, 16), op=ALU.mult), [bim, cre], [tc_])
        dv(lambda e: e.tensor_tensor(out=td.ap, in0=bre.ap, in1=bc(cim, 16), op=ALU.mult), [bre, cim], [td])
        lo, hi = slice(0, 64), slice(64, 128)
        dv(lambda e: e.tensor_tensor(out=X1.ap[lo], in0=ta.ap[lo], in1=tb_.ap[lo], op=ALU.subtract), [ta, tb_], [X1])
        dv(lambda e: e.tensor_tensor(out=X2.ap[lo], in0=tc_.ap[lo], in1=td.ap[lo], op=ALU.add), [tc_, td], [X2])
        dv(lambda e: e.tensor_tensor(out=X1.ap[hi], in0=tc_.ap[hi], in1=td.ap[hi], op=ALU.add), [tc_, td], [X1])
        dv(lambda e: e.tensor_tensor(out=X2.ap[hi], in0=tb_.ap[hi], in1=ta.ap[hi], op=ALU.subtract), [ta, tb_], [X2])
        Zp = [A.carve([G, 128], F32, "Zp%d" % i) for i in range(2)]
        Win = [A.carve([G, 128], BF16, "Win%d" % i) for i in range(2)]
        for i, X in enumerate((X1, X2)):
            P.op("pool", lambda e, i=i: e.memset(Zp[i].ap, 0.0), writes=[Zp[i].reg])
            for g in range(G):
                s0 = (g % 8) * 16
                P.op("dve", lambda e, i=i, g=g, s0=s0, X=X: e.tensor_copy(out=Zp[i].ap[:, g, s0:s0 + 16], in_=X.ap[:, g, :]),
                     reads=[X.reg], writes=[Zp[i].reg])
            for g in range(G):
                bk = banks[g % 2]
                P.op("pe", lambda e, bk=bk, i=i, g=g: e.transpose(out=bk.ap[:, 0:128], in_=Zp[i].ap[:, g, :], identity=ident_f.ap),
                     reads=[Zp[i].reg, ident_f.reg], writes=[bk.reg])
                P.op("act", lambda e, bk=bk, i=i, g=g: e.activation(out=Win[i].ap[:, g, :], in_=bk.ap[:, 0:128], func=AF.Copy),
                     reads=[bk.reg], writes=[Win[i].reg])
        cnat = [A.carve([2, 128], F32, "cnat%d" % i) for i in range(2)]
        CT = [A.carve([G, 16], F32, "CT%d" % i) for i in range(2)]
        for i, src in enumerate((s5_c_re, s5_c_im)):
            v = src[l].rearrange("g c p -> (g c) p").rearrange("(t r) p -> r t p", r=128)
            for dup in range(2):
                P.dma("sp", cnat[i].ap[:, :, dup * 64:(dup + 1) * 64], v, writes=[cnat[i].reg])
            for t in range(2):
                bk = banks[2 + t]
                P.op("pe", lambda e, bk=bk, i=i, t=t: e.transpose(out=bk.ap[:, 0:128], in_=cnat[i].ap[:, t, :], identity=ident_f.ap),
                     reads=[cnat[i].reg, ident_f.reg], writes=[bk.reg])
                P.op("act", lambda e, bk=bk, i=i, t=t: e.activation(
                    out=CT[i].ap[:, t * 8:(t + 1) * 8, :], in_=bk.ap[:, 0:128].rearrange("p (g c) -> p g c", g=8), func=AF.Copy),
                    reads=[bk.reg], writes=[CT[i].reg])
        Wo = [A.carve([G, 128], BF16, "Wo%d" % i) for i in range(2)]
        for i in range(2):
            P.op("pool", lambda e, i=i: e.memset(Wo[i].ap, 0.0), writes=[Wo[i].reg])
        for g in range(G):
            s0 = (g % 8) * 16
            P.op("dve", lambda e, g=g, s0=s0: e.tensor_copy(out=Wo[0].ap[lo, g, s0:s0 + 16], in_=CT[0].ap[lo, g, :]),
                 reads=[CT[0].reg], writes=[Wo[0].reg])
            P.op("dve", lambda e, g=g, s0=s0: e.tensor_scalar(out=Wo[0].ap[hi, g, s0:s0 + 16], in0=CT[1].ap[hi, g, :],
                                                               scalar1=-1.0, scalar2=None, op0=ALU.mult),
                 reads=[CT[1].reg], writes=[Wo[0].reg])
            P.op("dve", lambda e, g=g, s0=s0: e.tensor_scalar(out=Wo[1].ap[lo, g, s0:s0 + 16], in0=CT[1].ap[lo, g, :],
                                                               scalar1=-1.0, scalar2=None, op0=ALU.mult),
                 reads=[CT[1].reg], writes=[Wo[1].reg])
            P.op("dve", lambda e, g=g, s0=s0: e.tensor_scalar(out=Wo[1].ap[hi, g, s0:s0 + 16], in0=CT[0].ap[hi, g, :],
                                                               scalar1=-1.0, scalar2=None, op0=ALU.mult),
                 reads=[CT[0].reg], writes=[Wo[1].reg])
        cosT = A.carve([G, TT], F32, "cosT")
        sinT = A.carve([G, TT], F32, "sinT")
        P.op("pool", lambda e: e.memset(cosT.ap[:, :, 0:1], 1.0), writes=[cosT.reg])
        P.op("pool", lambda e: e.memset(sinT.ap[:, :, 0:1], 0.0), writes=[sinT.reg])
        w1 = A.carve([G, 128], F32, "w1")
        w2 = A.carve([G, 128], F32, "w2")
        cur, nxt = cs, cs2
        m = 1
        while m < TT:
            cj, sj = cur
            for off in range(0, m, 128):
                bl = min(m, 128)
                a0 = slice(off, off + bl)
                a1 = slice(m + off, m + off + bl)
                dv(lambda e, cj=cj, bl=bl, a0=a0: e.tensor_tensor(out=w1.ap[:, :, 0:bl], in0=cosT.ap[:, :, a0], in1=bc(cj, bl), op=ALU.mult), [cosT, cj], [w1])
                dv(lambda e, sj=sj, bl=bl, a0=a0: e.tensor_tensor(out=w2.ap[:, :, 0:bl], in0=sinT.ap[:, :, a0], in1=bc(sj, bl), op=ALU.mult), [sinT, sj], [w2])
                dv(lambda e, bl=bl, a1=a1: e.tensor_tensor(out=cosT.ap[:, :, a1], in0=w1.ap[:, :, 0:bl], in1=w2.ap[:, :, 0:bl], op=ALU.subtract), [w1, w2], [cosT])
                dv(lambda e, cj=cj, bl=bl, a0=a0: e.tensor_tensor(out=w1.ap[:, :, 0:bl], in0=sinT.ap[:, :, a0], in1=bc(cj, bl), op=ALU.mult), [sinT, cj], [w1])
                dv(lambda e, sj=sj, bl=bl, a0=a0: e.tensor_tensor(out=w2.ap[:, :, 0:bl], in0=cosT.ap[:, :, a0], in1=bc(sj, bl), op=ALU.mult), [cosT, sj], [w2])
                dv(lambda e, bl=bl, a1=a1: e.tensor_tensor(out=sinT.ap[:, :, a1], in0=w1.ap[:, :, 0:bl], in1=w2.ap[:, :, 0:bl], op=ALU.add), [w1, w2], [sinT])
            nc_, ns_ = nxt
            dv(lambda e, cj=cj: e.tensor_tensor(out=tmp.ap, in0=cj.ap, in1=cj.ap, op=ALU.mult), [cj], [tmp])
            dv(lambda e, sj=sj: e.tensor_tensor(out=tmp2.ap, in0=sj.ap, in1=sj.ap, op=ALU.mult), [sj], [tmp2])
            dv(lambda e, nc_=nc_: e.tensor_tensor(out=nc_.ap, in0=tmp.ap, in1=tmp2.ap, op=ALU.subtract), [tmp, tmp2], [nc_])
            dv(lambda e, ns_=ns_, cj=cj, sj=sj: e.scalar_tensor_tensor(out=ns_.ap, in0=cj.ap, scalar=2.0, in1=sj.ap, op0=ALU.mult, op1=ALU.mult), [cj, sj], [ns_])
            cur, nxt = nxt, cur
            m *= 2
        c9, s9 = cur
        ns9 = small("ns9")
        dv(lambda e: e.tensor_scalar(out=ns9.ap, in0=s9.ap, scalar1=-1.0, scalar2=None, op0=ALU.mult), [s9], [ns9])
        Mr = A.carve([G, 128], F32, "Mr")
        for (ph, csl, src, idsl) in ((slice(0, 128), slice(0, 128), c9, slice(0, 128)),
                                     (lo, slice(64, 128), s9, slice(0, 64)),
                                     (hi, slice(0, 64), ns9, slice(64, 128))):
            npart = ph.stop - ph.start
            ncol = csl.stop - csl.start
            P.op("dve", lambda e, ph=ph, csl=csl, src=src, idsl=idsl, npart=npart, ncol=ncol: e.tensor_tensor(
                out=Mr.ap[ph, :, csl],
                in0=ident_f.ap[ph, idsl].unsqueeze(1).to_broadcast([npart, G, ncol]),
                in1=src.ap[ph, :].unsqueeze(2).to_broadcast([npart, G, ncol]), op=ALU.mult),
                reads=[ident_f.reg, src.reg], writes=[Mr.reg])
        wglu = A.carve([2, 256], F32, "wglu")
        P.dma("sp", wglu.ap, s5_w_glu[l].rearrange("(ct p) j -> p ct j", p=128), writes=[wglu.reg])
        carry = small("carry")
        P.op("pool", lambda e: e.memset(carry.ap, 0.0), writes=[carry.reg])
        creg = [carry.reg.sub(g) for g in range(G)]
        NB = 3
        usf = [A.carve([2, TT], F32, "usf%d" % i) for i in range(2)]
        usb = [A.carve([2, TT], BF16, "usb%d" % i) for i in range(2)]
        t1 = [A.carve([TT], F32, "t1_%d" % i) for i in range(NB)]
        t2 = [A.carve([TT], F32, "t2_%d" % i) for i in range(NB)]
        qin = [A.carve([TT], F32, "qin%d" % i) for i in range(NB)]
        qq = [A.carve([TT], F32, "qq%d" % i) for i in range(NB)]
        Z1 = [A.carve([TT], BF16, "Z1_%d" % i) for i in range(NB)]
        Z2 = [A.carve([TT], BF16, "Z2_%d" % i) for i in range(NB)]
        y0 = A.carve([TT], F32, "y0")
        y1 = [A.carve([TT], F32, "y1_%d" % i) for i in range(2)]
        y2 = [A.carve([TT], F32, "y2_%d" % i) for i in range(2)]
        sq = [A.carve([TT], F32, "sqs5_%d" % i) for i in range(2)]
        sig = A.carve([TT], F32, "sig")
        sdc = A.carve([TT], F32, "sdc5")
        rstdc = A.carve([TT], F32, "rstdc5")
        mo = A.carve([2, TT], BF16, "mos5")
        N_IT = NT * 16
        deferred = {}

        def later(step, fn):
            deferred.setdefault(step, []).append(fn)

        def info(i):
            ch, r = divmod(i, 16)
            ct, gl = divmod(r, 8)
            return ch, ct, gl, ct * 8 + gl

        def load_chunk(ch):
            sl = slice(ch * TT, (ch + 1) * TT)
            uf, ub_ = usf[ch % 2], usb[ch % 2]
            P.dma("sp", uf.ap, rgsT.ap[512:768, sl].rearrange("(ct p) n -> p ct n", p=128), reads=[rgsT.reg], writes=[uf.reg])
            P.op("act", lambda e, uf=uf, ub_=ub_: e.activation(out=ub_.ap, in_=uf.ap, func=AF.Copy), reads=[uf.reg], writes=[ub_.reg])

        def S1(i):
            ch, ct, gl, g = info(i)
            ub_ = usb[ch % 2]
            p1, p2 = banks[(i % 2) * 2], banks[(i % 2) * 2 + 1]
            P.op("pe", lambda e: e.matmul(p1.ap, lhsT=Win[0].ap[:, g, :], rhs=ub_.ap[:, ct, :], start=True, stop=True),
                 reads=[Win[0].reg, ub_.reg], writes=[p1.reg])
            P.op("pe", lambda e: e.matmul(p2.ap, lhsT=Win[1].ap[:, g, :], rhs=ub_.ap[:, ct, :], start=True, stop=True),
                 reads=[Win[1].reg, ub_.reg], writes=[p2.reg])

        def S23(i):
            ch, ct, gl, g = info(i)
            b = i % NB
            p1, p2 = banks[(i % 2) * 2], banks[(i % 2) * 2 + 1]
            P.op("dve", lambda e: e.tensor_tensor(out=t1[b].ap, in0=p1.ap, in1=cosT.ap[:, g, :], op=ALU.mult),
                 reads=[p1.reg, cosT.reg], writes=[t1[b].reg])
            P.op("dve", lambda e: e.tensor_tensor(out=t2[b].ap, in0=p2.ap, in1=sinT.ap[:, g, :], op=ALU.mult),
                 reads=[p2.reg, sinT.reg], writes=[t2[b].reg])
            P.op("pool", lambda e: e.tensor_tensor(out=qin[b].ap, in0=t1[b].ap, in1=t2[b].ap, op=ALU.add),
                 reads=[t1[b].reg, t2[b].reg], writes=[qin[b].reg])

        def S456(i):
            ch, ct, gl, g = info(i)
            b = i % NB
            P.op("dve", lambda e: e.tensor_tensor_scan(
                out=qq[b].ap, data0=rr.ap[:, g:g + 1].to_broadcast([128, TT]), data1=qin[b].ap,
                initial=carry.ap[:, g:g + 1], op0=ALU.mult, op1=ALU.add),
                reads=[rr.reg, qin[b].reg, creg[g]], writes=[qq[b].reg])
            if ch < NT - 1:
                cbk = banks[6 + (i % 2)]
                P.op("pe", lambda e: e.matmul(cbk.ap[:, g:g + 1], lhsT=Mr.ap[:, g, :], rhs=qq[b].ap[:, TT - 1:TT], start=True, stop=True),
                     reads=[Mr.reg, qq[b].reg], writes=[cbk.reg])
                P.op("act", lambda e: e.activation(out=carry.ap[:, g:g + 1], in_=cbk.ap[:, g:g + 1], func=AF.Copy),
                     reads=[cbk.reg], writes=[creg[g]])
            P.op("dve", lambda e: e.tensor_tensor(out=Z1[b].ap, in0=qq[b].ap, in1=cosT.ap[:, g, :], op=ALU.mult),
                 reads=[qq[b].reg, cosT.reg], writes=[Z1[b].reg])
            P.op("pool", lambda e: e.tensor_tensor(out=Z2[b].ap, in0=qq[b].ap, in1=sinT.ap[:, g, :], op=ALU.mult),
                 reads=[qq[b].reg, sinT.reg], writes=[Z2[b].reg])

        def S7(i, k):
            ch, ct, gl, g = info(i)
            b = i % NB
            yb = banks[4 + ct]
            P.op("pe", lambda e: e.matmul(yb.ap, lhsT=Wo[0].ap[:, g, :], rhs=Z1[b].ap, start=(gl == 0), stop=False),
                 reads=[Wo[0].reg, Z1[b].reg], writes=[yb.reg])
            P.op("pe", lambda e: e.matmul(yb.ap, lhsT=Wo[1].ap[:, g, :], rhs=Z2[b].ap, start=False, stop=(gl == 7)),
                 reads=[Wo[1].reg, Z2[b].reg], writes=[yb.reg])
            if gl == 7:
                uf = usf[ch % 2]
                later(k + 1, lambda: post_ct(ch, ct, yb, uf))
                if ct == 1:
                    later(k + 2, lambda: post_a(ch))
                    later(k + 3, lambda: post_b(ch))
                    later(k + 4, lambda: post_c(ch))

        def post_ct(ch, ct, yb, uf):
            P.op("dve", lambda e: e.scalar_tensor_tensor(
                out=y0.ap, in0=uf.ap[:, ct, :], scalar=dvec.ap[:, ct:ct + 1], in1=yb.ap, op0=ALU.mult, op1=ALU.add),
                reads=[uf.reg, dvec.reg, yb.reg], writes=[y0.reg])
            P.op("act", lambda e: e.activation(out=y1[ct].ap, in_=y0.ap, func=AF.Gelu_apprx_tanh),
                 reads=[y0.reg], writes=[y1[ct].reg])

        def post_a(ch):
            for co in range(2):
                zb = banks[7]
                for ct in range(2):
                    P.op("pe", lambda e, ct=ct, co=co: e.matmul(zb.ap, lhsT=wglu.ap[:, ct, co * 128:(co + 1) * 128], rhs=y1[ct].ap,
                                                                start=(ct == 0), stop=(ct == 1)),
                         reads=[wglu.reg, y1[ct].reg], writes=[zb.reg])
                P.op("act", lambda e, co=co: e.activation(out=sig.ap if co == 0 else sdc.ap, in_=zb.ap, func=AF.Sigmoid),
                     reads=[zb.reg], writes=[sig.reg if co == 0 else sdc.reg])

        def post_b(ch):
            for co in range(2):
                sg = sig if co == 0 else sdc
                P.op("dve", lambda e, co=co, sg=sg: e.tensor_tensor(out=y2[co].ap, in0=y1[co].ap, in1=sg.ap, op=ALU.mult),
                     reads=[y1[co].reg, sg.reg], writes=[y2[co].reg])
                P.op("act", lambda e, co=co: e.activation(out=sq[co].ap, in_=y2[co].ap, func=AF.Square),
                     reads=[y2[co].reg], writes=[sq[co].reg])
            vb = banks[7]
            for co in range(2):
                P.op("pe", lambda e, co=co: e.matmul(vb.ap, lhsT=ones_f.ap, rhs=sq[co].ap, start=(co == 0), stop=(co == 1)),
                     reads=[ones_f.reg, sq[co].reg], writes=[vb.reg])
            P.op("act", lambda e: e.activation(out=sdc.ap, in_=vb.ap, func=AF.Sqrt, scale=1.0 / 256, bias=EPS),
                 reads=[vb.reg], writes=[sdc.reg])

        def post_c(ch):
            sl = slice(ch * TT, (ch + 1) * TT)
            P.op("dve", lambda e: e.reciprocal(out=rstdc.ap, in_=sdc.ap), reads=[sdc.reg], writes=[rstdc.reg])
            for co in range(2):
                P.op("dve", lambda e, co=co: e.scalar_tensor_tensor(
                    out=mo.ap[:, co, :], in0=y2[co].ap, scalar=gg.ap[:, co:co + 1], in1=rstdc.ap, op0=ALU.mult, op1=ALU.mult),
                    reads=[y2[co].reg, gg.reg, rstdc.reg], writes=[mo.reg])
            P.dma("sp", mixedT.ap[768:1024, sl].rearrange("(c p) n -> p c n", p=128), mo.ap, reads=[mo.reg], writes=[mixedT.reg])

        load_chunk(0)
        for k in range(N_IT + 8):
            if k % 4 == 1:
                pop_cast()
            if k < N_IT:
                if k % 16 == 8 and k // 16 + 1 < NT:
                    load_chunk(k // 16 + 1)
                S1(k)
            if 0 <= k - 1 < N_IT:
                S23(k - 1)
            if 0 <= k - 2 < N_IT:
                S456(k - 2)
            if 0 <= k - 3 < N_IT:
                S7(k - 3, k)
            for fn in deferred.pop(k, []):
                fn()
        assert not deferred

    xres_t = [xres.reg.sub(t) for t in range(NT)]

    def phase_E(l, xsrc, xsrc_regs):
        A.top = A_BASE
        wo = A.carve([8, D], BF16, "wo")
        P.dma("sp", wo.ap, w_out_b[l].ap.rearrange("(c p) n -> p c n", p=128), reads=[w_out_b[l].reg], writes=[wo.reg])
        mts = [A.carve([8, TT], BF16, "mt%d" % i) for i in range(2)]
        xts = [A.carve([4, D], F32, "xte%d" % i) for i in range(2)]
        k = 0
        for t in range(NT):
            mt, xt = mts[t % 2], xts[t % 2]
            P.dma("sp", mt.ap, mixedT.ap[:, t * TT:(t + 1) * TT].rearrange("(c p) n -> p c n", p=128),
                  reads=[mixedT.reg], writes=[mt.reg])
            P.dma("sp", xt.ap, xsrc[t * TT:(t + 1) * TT, :].rearrange("(s p) d -> p s d", p=128),
                  reads=[xsrc_regs[t]], writes=[xt.reg])
            for s in range(4):
                for half in range(2):
                    bk = banks[k % 8]
                    k += 1
                    for c in range(8):
                        P.op("pe", lambda e, bk=bk, mt=mt, c=c, s=s, half=half: e.matmul(
                            bk.ap, lhsT=mt.ap[:, c, s * 128:(s + 1) * 128], rhs=wo.ap[:, c, half * 512:(half + 1) * 512],
                            start=(c == 0), stop=(c == 7)), reads=[mt.reg, wo.reg], writes=[bk.reg])
                    P.op("dve", lambda e, bk=bk, xt=xt, s=s, half=half: e.tensor_tensor(
                        out=xt.ap[:, s, half * 512:(half + 1) * 512], in0=bk.ap, in1=xt.ap[:, s, half * 512:(half + 1) * 512],
                        op=ALU.add), reads=[bk.reg, xt.reg], writes=[xt.reg])
            P.dma("sp", xres.ap[t * TT:(t + 1) * TT, :].rearrange("(s p) d -> p s d", p=128), xt.ap,
                  reads=[xt.reg], writes=[xres_t[t]])

    def phase_F(l, moe):
        A.top = A_BASE
        E = NE if moe else 1
        w1s = mw1_b if moe else fw1_b
        w3s = mw3_b if moe else fw3_b
        w2s = mw2_b if moe else fw2_b
        g_rep = A.carve([1, D], F32, "gf_rep")
        P.dma("sp", g_rep.ap, norm_ffn_g[l:l + 1, :].partition_broadcast(128), writes=[g_rep.reg])
        if moe:
            gfin = A.carve([1, D], F32, "gfin")
            P.dma("sp", gfin.ap, final_norm_g[0:1, :].partition_broadcast(128), writes=[gfin.reg])
            Rsb = A.carve([8, NE], F32, "Rsb")
            import os as _os3
            if not _os3.environ.get("K_NORSB"):
                P.dma("sp", Rsb.ap, moe_router[0].rearrange("(c p) e -> p c e", p=128), writes=[Rsb.reg])
            hT32 = A.carve([8, TT], F32, "hT32")
            idxs = A.carve([16], U32, "idxs")
            P.dma("sp", idxs.ap, tok_idx, writes=[idxs.reg])
            Gt = A.carve([4, NE], F32, "Gt")
            lgs, eq1, eq2, lg2 = [A.carve([NE], F32, n) for n in ("lgs", "eq1", "eq2", "lg2")]
            m1, m2, dd, g1, g2 = [A.carve([1], F32, n) for n in ("m1", "m2", "dd", "g1", "g2")]
        xt = A.carve([4, D], F32, "xtf")
        h32 = A.carve([4, D], F32, "h32")
        hT = A.carve([8, TT], BF16, "hT")
        actT = A.carve([NF, TT], BF16, "actT")
        W2s = [A.carve([NF, D], BF16, "W2s0")]
        GT = 4
        NW = 3
        w13 = [A.carve([8, 2, GT * 128], BF16, "w13_%d" % i) for i in range(NW)]
        w13r = [(w.reg.sub("w1"), w.reg.sub("w3")) for w in w13]
        sa = [A.carve([TT], BF16, "sa%d" % i) for i in range(2)]
        junk = A.carve([D], BF16, "junkf")
        ss, sd, rstd = [A.carve([4], F32, n) for n in ("ssf", "sdf", "rstdf")]

        import os as _os
        _nt = int(_os.environ.get("K_NT_F", NT // 2)) if moe else NT
        _ne = int(_os.environ.get("K_NE", E)) if moe else E
        iters = [(t, e_) for t in range(_nt) for e_ in range(_ne)]
        E = _ne
        NG = (NF + GT - 1) // GT
        glist = [(i_, g_) for i_ in range(len(iters)) for g_ in range(NG)]

        def load_w2(i):
            t, e_ = iters[i]
            w = W2s[0]
            P.dma("sp", w.ap, w2s[e_].ap.rearrange("(j p) n -> p j n", p=128), reads=[w2s[e_].reg], writes=[w.reg])

        gcount = [0]

        def load_w13(gi):
            i, grp = glist[gi]
            t, e_ = iters[i]
            w = w13[gi % NW]
            c0 = grp * GT * 128
            c1 = min(FF, c0 + GT * 128)
            P.dma("sp", w.ap[:, :, 0, 0:c1 - c0], w1s[e_].ap[:, c0:c1].rearrange("(c p) f -> p c f", p=128),
                  reads=[w1s[e_].reg], writes=[w13r[gi % NW][0]])
            P.dma("sp", w.ap[:, :, 1, 0:c1 - c0], w3s[e_].ap[:, c0:c1].rearrange("(c p) f -> p c f", p=128),
                  reads=[w3s[e_].reg], writes=[w13r[gi % NW][1]])

        load_w13(0)
        load_w13(1)
        kab = 0
        kw2 = 0
        w2banks = (0, 1, 6, 7)
        for i, (t, e_) in enumerate(iters):
            if e_ == 0:
                if moe:
                    for s in range(4):
                        P._add("pool", lambda e, s=s, t=t: e.indirect_dma_start(
                            out=xt.ap[:, s, :], out_offset=None, in_=xres.ap[:, :],
                            in_offset=bass.IndirectOffsetOnAxis(ap=idxs.ap[:, t * 4 + s:t * 4 + s + 1], axis=0)),
                            [xres.reg, idxs.reg], [xt.reg], True)
                else:
                    P.dma("sp", xt.ap, xres.ap[t * TT:(t + 1) * TT, :].rearrange("(s p) d -> p s d", p=128),
                          reads=[xres_t[t]], writes=[xt.reg])
                for s in range(4):
                    rmsnorm_rows(xt, s, ss, sd, rstd, junk, D)
                P.op("act", lambda e: e.activation(out=sd.ap, in_=ss.ap, func=AF.Sqrt, scale=1.0 / D, bias=EPS),
                     reads=[ss.reg], writes=[sd.reg])
                P.op("dve", lambda e: e.reciprocal(out=rstd.ap, in_=sd.ap), reads=[sd.reg], writes=[rstd.reg])
                for s in range(4):
                    P.op("dve", lambda e, s=s: e.scalar_tensor_tensor(
                        out=h32.ap[:, s, :], in0=xt.ap[:, s, :], scalar=rstd.ap[:, s:s + 1], in1=g_rep.ap[:, 0, :],
                        op0=ALU.mult, op1=ALU.mult), reads=[xt.reg, rstd.reg, g_rep.reg], writes=[h32.reg])
                for c in range(8):
                    bk = banks[c % 2]
                    for s in range(4):
                        P.op("pe", lambda e, bk=bk, s=s, c=c: e.transpose(
                            out=bk.ap[:, s * 128:(s + 1) * 128], in_=h32.ap[:, s, c * 128:(c + 1) * 128],
                            identity=ident_f.ap), reads=[h32.reg, ident_f.reg], writes=[bk.reg])
                    P.op("act", lambda e, bk=bk, c=c: e.activation(out=hT.ap[:, c, :], in_=bk.ap, func=AF.Copy),
                         reads=[bk.reg], writes=[hT.reg])
                    if moe:
                        P.op("dve", lambda e, bk=bk, c=c: e.tensor_copy(out=hT32.ap[:, c, :], in_=bk.ap),
                             reads=[bk.reg], writes=[hT32.reg, bk.reg])
                _skip = _os.environ.get("K_SKIP", "")
                if moe and _skip:
                    P.op("dve", lambda e: e.memset(Gt.ap, 0.125), writes=[Gt.reg])
                if moe and not _skip:
                    lb = banks[2]
                    for s in range(4):
                        for c in range(8):
                            P.op("pe", lambda e, s=s, c=c: e.matmul(
                                lb.ap[:, s * 8:(s + 1) * 8], lhsT=hT32.ap[:, c, s * 128:(s + 1) * 128], rhs=Rsb.ap[:, c, :],
                                start=(c == 0), stop=(c == 7)), reads=[hT32.reg, Rsb.reg], writes=[lb.reg])
                        P.op("dve", lambda e, s=s: e.tensor_copy(out=lgs.ap, in_=lb.ap[:, s * 8:(s + 1) * 8]),
                             reads=[lb.reg], writes=[lgs.reg])
                        P.op("dve", lambda e: e.reduce_max(out=m1.ap, in_=lgs.ap, axis=AX.X), reads=[lgs.reg], writes=[m1.reg])
                        P.op("dve", lambda e: e.tensor_scalar(out=eq1.ap, in0=lgs.ap, scalar1=m1.ap[:, 0:1], scalar2=None,
                                                              op0=ALU.is_equal), reads=[lgs.reg, m1.reg], writes=[eq1.reg])
                        P.op("dve", lambda e: e.scalar_tensor_tensor(out=lg2.ap, in0=eq1.ap, scalar=-1e30, in1=lgs.ap,
                                                                     op0=ALU.mult, op1=ALU.add),
                             reads=[eq1.reg, lgs.reg], writes=[lg2.reg])
                        P.op("dve", lambda e: e.reduce_max(out=m2.ap, in_=lg2.ap, axis=AX.X), reads=[lg2.reg], writes=[m2.reg])
                        P.op("dve", lambda e: e.tensor_scalar(out=eq2.ap, in0=lg2.ap, scalar1=m2.ap[:, 0:1], scalar2=None,
                                                              op0=ALU.is_equal), reads=[lg2.reg, m2.reg], writes=[eq2.reg])
                        P.op("dve", lambda e: e.tensor_tensor(out=dd.ap, in0=m2.ap, in1=m1.ap, op=ALU.subtract),
                             reads=[m1.reg, m2.reg], writes=[dd.reg])
                        P.op("act", lambda e: e.activation(out=g2.ap, in_=dd.ap, func=AF.Sigmoid), reads=[dd.reg], writes=[g2.reg])
                        P.op("act", lambda e: e.activation(out=g1.ap, in_=dd.ap, func=AF.Sigmoid, scale=-1.0),
                             reads=[dd.reg], writes=[g1.reg])
                        P.op("dve", lambda e, s=s: e.tensor_scalar(out=Gt.ap[:, s, :], in0=eq1.ap, scalar1=g1.ap[:, 0:1], scalar2=None,
                                                                   op0=ALU.mult), reads=[eq1.reg, g1.reg], writes=[Gt.reg])
                        P.op("dve", lambda e, s=s: e.scalar_tensor_tensor(out=Gt.ap[:, s, :], in0=eq2.ap, scalar=g2.ap[:, 0:1],
                                                                          in1=Gt.ap[:, s, :], op0=ALU.mult, op1=ALU.add),
                             reads=[eq2.reg, g2.reg, Gt.reg], writes=[Gt.reg])
            _stop = int(_os.environ.get("K_STOP", "9")) if moe else 9
            if _stop <= 1:
                continue
            load_w2(i)
            W2 = W2s[0]
            for grp in range(NG):
                gi = i * NG + grp
                if gi + 2 < len(glist):
                    load_w13(gi + 2)
                w = w13[gi % NW]
                wr1, wr3 = w13r[gi % NW]
                for jj in range(min(GT, NF - grp * GT)):
                    j = grp * GT + jj
                    ba_, bb_ = banks[2 + (kab % 2) * 2], banks[3 + (kab % 2) * 2]
                    sab = sa[kab % 2]
                    kab += 1
                    for c in range(8):
                        P.op("pe", lambda e, ba_=ba_, w=w, c=c, jj=jj: e.matmul(
                            ba_.ap, lhsT=w.ap[:, c, 0, jj * 128:(jj + 1) * 128], rhs=hT.ap[:, c, :],
                            start=(c == 0), stop=(c == 7)), reads=[wr1, hT.reg], writes=[ba_.reg])
                    for c in range(8):
                        P.op("pe", lambda e, bb_=bb_, w=w, c=c, jj=jj: e.matmul(
                            bb_.ap, lhsT=w.ap[:, c, 1, jj * 128:(jj + 1) * 128], rhs=hT.ap[:, c, :],
                            start=(c == 0), stop=(c == 7)), reads=[wr3, hT.reg], writes=[bb_.reg])
                    P.op("act", lambda e, ba_=ba_, sab=sab: e.activation(out=sab.ap, in_=ba_.ap, func=AF.Silu),
                         reads=[ba_.reg], writes=[sab.reg])
                    P.op("dve", lambda e, bb_=bb_, sab=sab, j=j: e.tensor_tensor(
                        out=actT.ap[:, j, :], in0=bb_.ap, in1=sab.ap, op=ALU.mult),
                        reads=[bb_.reg, sab.reg], writes=[actT.reg])
            if _stop <= 2:
                continue
            for s in range(4):
                for half in range(2):
                    bk = banks[w2banks[kw2 % 4]]
                    kw2 += 1
                    for j in range(NF):
                        P.op("pe", lambda e, bk=bk, j=j, s=s, half=half, W2=W2: e.matmul(
                            bk.ap, lhsT=actT.ap[:, j, s * 128:(s + 1) * 128], rhs=W2.ap[:, j, half * 512:(half + 1) * 512],
                            start=(j == 0), stop=(j == NF - 1)), reads=[actT.reg, W2.reg], writes=[bk.reg])
                    xs = xt.ap[:, s, half * 512:(half + 1) * 512]
                    if moe:
                        P.op("dve", lambda e, bk=bk, xs=xs, s=s, e_=e_: e.scalar_tensor_tensor(
                            out=xs, in0=bk.ap, scalar=Gt.ap[:, s, e_:e_ + 1], in1=xs, op0=ALU.mult, op1=ALU.add),
                            reads=[bk.reg, Gt.reg, xt.reg], writes=[xt.reg])
                    else:
                        P.op("dve", lambda e, bk=bk, xs=xs: e.tensor_tensor(out=xs, in0=bk.ap, in1=xs, op=ALU.add),
                             reads=[bk.reg, xt.reg], writes=[xt.reg])
            if _stop <= 3:
                continue
            if e_ == E - 1:
                if not moe:
                    P.dma("sp", xres.ap[t * TT:(t + 1) * TT, :].rearrange("(s p) d -> p s d", p=128), xt.ap,
                          reads=[xt.reg], writes=[xres_t[t]])
                else:
                    for s in range(4):
                        rmsnorm_rows(xt, s, ss, sd, rstd, junk, D)
                    P.op("act", lambda e: e.activation(out=sd.ap, in_=ss.ap, func=AF.Sqrt, scale=1.0 / D, bias=EPS),
                         reads=[ss.reg], writes=[sd.reg])
                    P.op("dve", lambda e: e.reciprocal(out=rstd.ap, in_=sd.ap), reads=[sd.reg], writes=[rstd.reg])
                    for s in range(4):
                        P.op("dve", lambda e, s=s: e.scalar_tensor_tensor(
                            out=h32.ap[:, s, :], in0=xt.ap[:, s, :], scalar=rstd.ap[:, s:s + 1], in1=gfin.ap[:, 0, :],
                            op0=ALU.mult, op1=ALU.mult), reads=[xt.reg, rstd.reg, gfin.reg], writes=[h32.reg])
                    o = P.dma("sp", out_t.ap[t * TT:(t + 1) * TT, :].rearrange("(s p) d -> p s d", p=128), h32.ap,
                              reads=[h32.reg], writes=[out_t.reg])
                    state["out_ops"].append(o)

    ORDER = ["A0", "B0", "C0", "D0", "E0", "F0", "A1", "B1", "C1", "D1", "E1", "F1"]
    last = len(ORDER) - 1 if upto == "all" else ORDER.index(upto)
    x_in_regs = [Reg("x_in")] * NT
    for l in range(2):
        xsrc = x_in if l == 0 else xres.ap
        xregs = x_in_regs if l == 0 else xres_t
        xreg_whole = x_in_regs[0] if l == 0 else xres.reg
        if ORDER.index("A%d" % l) <= last:
            qT, kT, vaug, keep_top = phase_A(l, xsrc, xreg_whole)
            P.barrier()
        if ORDER.index("B%d" % l) <= last:
            phase_B(l, qT, kT, vaug, keep_top, casts=(l == 0 and last > ORDER.index("B0")))
            P.barrier()
        if ORDER.index("D%d" % l) <= last:
            phase_D(l)
            while cast_queue and last > ORDER.index("D0"):
                pop_cast()
            P.barrier()
        if ORDER.index("E%d" % l) <= last:
            phase_E(l, xsrc, xregs)
            P.barrier()
        if ORDER.index("F%d" % l) <= last:
            phase_F(l, moe=(l == 1))
            P.barrier()
    finals = state["out_ops"] if state["out_ops"] else [o for o in P.ops if o.is_dma][-24:]
    import os as _os2
    if _os2.environ.get("K_WAITCAST"):
        finals = list(finals) + list(P.dma_ops["pool"])
    P.emit(final_wait_ops=finals)
    es.close()
    return nc


def _make_biasT(rel_bias):
    ki = np.arange(128)[:, None, None]
    j = np.arange(5)[None, :, None]
    qi = np.arange(128)[None, None, :]
    rel = (4 - j) * 128 + qi - ki
    idx = np.clip(rel, -128, 128) + 128
    dc = 2 * (j - 4) + (ki >= 64).astype(np.int64) - (qi >= 64).astype(np.int64)
    valid = (dc >= -8) & (dc <= 0)
    ext = np.concatenate([rel_bias, np.full(rel_bias.shape[:2] + (1,), NEG, np.float32)], axis=-1)
    idx = np.where(valid, idx, 257)
    return np.ascontiguousarray(ext[:, :, idx].reshape(2, 8, 128, 640)).astype(np.float32)


def kernel(**inputs):
    nc = build()
    x = np.asarray(inputs["x"], dtype=np.float32)
    common = {}
    for k, v in inputs.items():
        if k in ("x", "attn_rel_bias", "final_norm_g"):
            continue
        common[k] = np.ascontiguousarray(np.asarray(v, dtype=np.float32))
    common["biasT"] = _make_biasT(np.asarray(inputs["attn_rel_bias"], dtype=np.float32))
    common["final_norm_g"] = np.ascontiguousarray(np.asarray(inputs["final_norm_g"], dtype=np.float32).reshape(1, D))
    n = 8
    in_maps = []
    for c in range(n):
        half = c // 4
        idx = (half * (L // 2) + np.arange(16)[None, :] * 128 + np.arange(128)[:, None]).astype(np.uint32)
        in_maps.append(dict(common, x=np.ascontiguousarray(x[c % 4]), tok_idx=np.ascontiguousarray(idx)))
    res = run_bass_kernel_spmd(nc, in_maps, core_ids=list(range(n)))
    out = np.empty((4, L, D), np.float32)
    for c in range(n):
        half = c // 4
        out[c % 4, half * (L // 2):(half + 1) * (L // 2)] = np.asarray(res.results[c]["out"], dtype=np.float32)
    return out
```
